# Optimizing a Trainium2 kernel written in Bass

```python
import math
import jax, jax.numpy as jnp
from jax import lax
import numpy as np

D_MODEL = 1024
BATCH = 8
SEQ = 2048
DEPTH = 2
DEC_BATCH = 128
DEC_SEQ = 4
PAST_LEN = 2048
PAGE_SIZE = 128

N_META = 16
HG_HEADS = 8
HG_DK = D_MODEL // HG_HEADS
HG_DV = D_MODEL // HG_HEADS
HG_CHUNK = 64
AT_HEADS = 8
AT_DQK = D_MODEL // (2 * AT_HEADS)
AT_DV = D_MODEL // AT_HEADS
Q_BLOCK = 128
N_BUCKETS = 32
MAX_DISTANCE = 128
D_FF = 256 * ((8 * D_MODEL // 3 + 255) // 256)
N_EXPERTS = 8
TOP_K = 2
D_FF_EXPERT = 7 * D_MODEL // 2
N_EVEN = (DEPTH + 1) // 2
N_ODD = DEPTH // 2
LEAD = ((N_META + Q_BLOCK - 1) // Q_BLOCK) * Q_BLOCK
EPS = 1e-6
NEG = -1e30

kernel_name = 'hgrn2_diffattn_hybrid_step'


def rmsnorm(x, w):
    xf = x.astype(jnp.float32)
    y = xf * lax.rsqrt(jnp.mean(xf * xf, axis=-1, keepdims=True) + EPS)
    return (y * w.astype(jnp.float32)).astype(x.dtype)


def rel_bias(dist, table):
    n = jnp.maximum(dist, 0)
    max_exact = N_BUCKETS // 2
    nf = jnp.maximum(n, 1).astype(jnp.float32)
    large = max_exact + (jnp.log(nf / max_exact) / math.log(MAX_DISTANCE / max_exact)
                         * (N_BUCKETS - max_exact)).astype(jnp.int32)
    bucket = jnp.where(n < max_exact, n, jnp.minimum(large, N_BUCKETS - 1))
    return jnp.moveaxis(table[bucket].astype(jnp.float32), -1, 0)


def attn_partial(q, k, v, bias, mask):
    s = jnp.einsum('bqhmd,bkhmd->bhmqk', q, k).astype(jnp.float32) + bias[None, :, None]
    if mask is not None:
        s = jnp.where(mask[:, None, None], s, NEG)
    m = jnp.max(s, axis=-1)
    p = jnp.exp(s - m[..., None])
    l = jnp.sum(p, axis=-1)
    acc = jnp.einsum('bhmqk,bkhd->bhmqd', p, v.astype(jnp.float32))
    return m, l, acc


def attn_merge(a, b):
    m = jnp.maximum(a[0], b[0])
    ea = jnp.exp(a[0] - m)
    eb = jnp.exp(b[0] - m)
    return m, a[1] * ea + b[1] * eb, a[2] * ea[..., None] + b[2] * eb[..., None]


def diff_finalize(stats, lam, lambda_init, subln_w):
    _, l, acc = stats
    o = acc / l[..., None]
    o = o[:, :, 0] - lam * o[:, :, 1]
    o = rmsnorm(o, subln_w) * (1.0 - lambda_init)
    return jnp.transpose(o, (0, 2, 1, 3))


def diff_qkv(h, w_qkv):
    B, T, _ = h.shape
    q, k, v = jnp.split(h @ w_qkv, 3, axis=-1)
    q = q.reshape(B, T, AT_HEADS, 2, AT_DQK) * (AT_DQK ** -0.5)
    k = k.reshape(B, T, AT_HEADS, 2, AT_DQK)
    v = v.reshape(B, T, AT_HEADS, AT_DV)
    return q, k, v


def diff_lambda(lq1, lk1, lq2, lk2, lambda_init):
    f = jnp.float32
    return (jnp.exp(jnp.sum(lq1.astype(f) * lk1.astype(f)))
            - jnp.exp(jnp.sum(lq2.astype(f) * lk2.astype(f))) + lambda_init)


def diff_attn_prompt(h, pos, valid, w_qkv, w_out, lam, lambda_init, subln_w, table):
    B, L, _ = h.shape
    q, k, v = diff_qkv(h, w_qkv)
    nb = L // Q_BLOCK
    qb = jnp.swapaxes(q.reshape(B, nb, Q_BLOCK, AT_HEADS, 2, AT_DQK), 0, 1)
    qpos = pos.reshape(nb, Q_BLOCK)

    def block(args):
        qi, qp = args
        bias = rel_bias(qp[:, None] - pos[None, :], table)
        mask = (pos[None, :] <= qp[:, None]) & valid[None, :]
        return diff_finalize(attn_partial(qi, k, v, bias, mask[None]), lam, lambda_init, subln_w)

    o = jnp.swapaxes(lax.map(block, (qb, qpos)), 0, 1).reshape(B, L, D_MODEL)
    return o.astype(h.dtype) @ w_out, k, v


def diff_attn_sample(h, a, cache_k, cache_v, page_table, w_qkv, w_out, lam, lambda_init, subln_w, table):
    DB, T, _ = h.shape
    q, k, v = diff_qkv(h, w_qkv)
    n_pages = page_table.shape[1]
    qpos = PAST_LEN + jnp.arange(T)
    offs = jnp.arange(PAGE_SIZE)

    def page_step(carry, xs):
        pids, p = xs
        kp = cache_k[a, pids]
        vp = cache_v[a, pids]
        bias = rel_bias(qpos[:, None] - (p * PAGE_SIZE + offs)[None, :], table)
        return attn_merge(carry, attn_partial(q, kp, vp, bias, None)), None

    init = (jnp.full((DB, AT_HEADS, 2, T), NEG, jnp.float32),
            jnp.zeros((DB, AT_HEADS, 2, T), jnp.float32),
            jnp.zeros((DB, AT_HEADS, 2, T, AT_DV), jnp.float32))
    stats, _ = lax.scan(page_step, init, (page_table.T, jnp.arange(n_pages)))
    bias = rel_bias(qpos[:, None] - qpos[None, :], table)
    causal = jnp.tril(jnp.ones((T, T), bool))
    stats = attn_merge(stats, attn_partial(q, k, v, bias, causal[None]))
    o = diff_finalize(stats, lam, lambda_init, subln_w).reshape(DB, T, D_MODEL)
    return o.astype(h.dtype) @ w_out, k, v


def hgrn_proj(h, w_in, lb):
    B, T, _ = h.shape
    q, f, i, gate = jnp.split(h @ w_in, 4, axis=-1)
    forget = lb + (1.0 - lb) * jax.nn.sigmoid(f.astype(jnp.float32))
    shp = (B, T, HG_HEADS, HG_DK)
    q = jax.nn.silu(q.astype(jnp.float32)).reshape(shp)
    k = (1.0 - forget).reshape(shp)
    g = jnp.log(forget).reshape(shp)
    v = i.astype(jnp.float32).reshape(B, T, HG_HEADS, HG_DV)
    return q, k, v, g, gate


def hgrn_chunk(S, q, k, v, g):
    C = q.shape[1]
    G = jnp.cumsum(g, axis=1)
    causal = jnp.tril(jnp.ones((C, C), bool))[None, :, :, None, None]
    decay = jnp.exp(jnp.where(causal, G[:, :, None] - G[:, None, :], NEG))
    A = jnp.einsum('bthd,btshd,bshd->bhts', q, decay, k)
    o = (jnp.einsum('bhts,bshv->bthv', A, v)
         + jnp.einsum('bthd,bhdv->bthv', q * jnp.exp(G), S))
    G_end = G[:, -1]
    S_new = (jnp.exp(G_end)[..., None] * S
             + jnp.einsum('bshd,bshv->bhdv', k * jnp.exp(G_end[:, None] - G), v))
    return S_new, o


def hgrn_out(o, gate, norm_w, w_out, dtype):
    B, T = o.shape[:2]
    o = o.reshape(B, T, D_MODEL) * jax.nn.sigmoid(gate.astype(jnp.float32))
    return rmsnorm(o, norm_w).astype(dtype) @ w_out


def hgrn_prompt(h, valid, w_in, lb, norm_w, w_out):
    B, L, _ = h.shape
    q, k, v, g, gate = hgrn_proj(h, w_in, lb)
    vm = valid[None, :, None, None]
    k = jnp.where(vm, k, 0.0)
    g = jnp.where(vm, g, 0.0)
    nc = L // HG_CHUNK

    def to_chunks(t):
        return jnp.swapaxes(t.reshape(B, nc, HG_CHUNK, *t.shape[2:]), 0, 1)

    S0 = jnp.zeros((B, HG_HEADS, HG_DK, HG_DV), jnp.float32)
    S, o = lax.scan(lambda S, xs: hgrn_chunk(S, *xs), S0,
                    (to_chunks(q), to_chunks(k), to_chunks(v), to_chunks(g)))
    o = jnp.swapaxes(o, 0, 1).reshape(B, L, HG_HEADS, HG_DV)
    return hgrn_out(o, gate, norm_w, w_out, h.dtype), S


def hgrn_sample(h, S0, w_in, lb, norm_w, w_out):
    q, k, v, g, gate = hgrn_proj(h, w_in, lb)
    S, o = hgrn_chunk(S0.astype(jnp.float32), q, k, v, g)
    return hgrn_out(o, gate, norm_w, w_out, h.dtype), S


def swiglu(h, w_up, w_down):
    a, b = jnp.split(h @ w_up, 2, axis=-1)
    return (jax.nn.silu(a) * b) @ w_down


def moe_swiglu(h, w_router, b_router, w_up, w_down):
    logits = (h @ w_router).astype(jnp.float32) + b_router.astype(jnp.float32)
    top_v, top_i = lax.top_k(logits, TOP_K)
    gates = jax.nn.softmax(top_v, axis=-1)
    dense = jnp.sum(jax.nn.one_hot(top_i, N_EXPERTS, dtype=jnp.float32) * gates[..., None], axis=-2)
    out = jnp.zeros(h.shape, jnp.float32)
    for e in range(N_EXPERTS):
        out = out + dense[..., e:e + 1] * swiglu(h, w_up[e], w_down[e]).astype(jnp.float32)
    return out.astype(h.dtype)


def setup_inputs(seed: int = 0) -> dict:
    key = jax.random.key(seed)
    ks = jax.random.split(key, 32)
    f32 = jnp.float32
    n_pages = PAST_LEN // PAGE_SIZE
    n_used = DEC_BATCH * n_pages
    n_phys = n_used + max(1, n_used // 4)

    def nrm(k, shape, scale):
        return jax.random.normal(k, shape, f32) * scale

    def gain(k, shape):
        return 1.0 + 0.01 * jax.random.normal(k, shape, f32)

    page_table = jax.random.permutation(ks[5], n_phys)[:n_used].reshape(DEC_BATCH, n_pages).astype(jnp.int32)
    return {
        'x_prompt': nrm(ks[0], (BATCH, SEQ, D_MODEL), 1.0),
        'x_sample': nrm(ks[1], (DEC_BATCH, DEC_SEQ, D_MODEL), 1.0),
        'state_hgrn': nrm(ks[2], (N_EVEN, DEC_BATCH, HG_HEADS, HG_DK, HG_DV), 0.1),
        'cache_k': nrm(ks[3], (N_ODD, n_phys, PAGE_SIZE, AT_HEADS, 2, AT_DQK), 1.0),
        'cache_v': nrm(ks[4], (N_ODD, n_phys, PAGE_SIZE, AT_HEADS, AT_DV), 1.0),
        'page_table': page_table,
        'meta_tokens': nrm(ks[6], (N_META, D_MODEL), 1.0),
        'norm_mix_w': gain(ks[7], (DEPTH, D_MODEL)),
        'norm_ffn_w': gain(ks[8], (DEPTH, D_MODEL)),
        'hg_w_in': nrm(ks[9], (N_EVEN, D_MODEL, 4 * D_MODEL), D_MODEL ** -0.5),
        'hg_lower_bound': nrm(ks[10], (N_EVEN + 1, D_MODEL), 0.5),
        'hg_norm_w': gain(ks[11], (N_EVEN, D_MODEL)),
        'hg_w_out': nrm(ks[12], (N_EVEN, D_MODEL, D_MODEL), D_MODEL ** -0.5),
        'at_w_qkv': nrm(ks[13], (N_ODD, D_MODEL, 3 * D_MODEL), D_MODEL ** -0.5),
        'at_lambda_q1': nrm(ks[14], (N_ODD, AT_DQK), 0.1),
        'at_lambda_k1': nrm(ks[15], (N_ODD, AT_DQK), 0.1),
        'at_lambda_q2': nrm(ks[16], (N_ODD, AT_DQK), 0.1),
        'at_lambda_k2': nrm(ks[17], (N_ODD, AT_DQK), 0.1),
        'at_subln_w': gain(ks[18], (N_ODD, AT_DV)),
        'at_w_out': nrm(ks[19], (N_ODD, D_MODEL, D_MODEL), D_MODEL ** -0.5),
        'rel_bias_table': nrm(ks[20], (N_BUCKETS, AT_HEADS), 0.5),
        'ff_w_up': nrm(ks[21], (N_EVEN, D_MODEL, 2 * D_FF), D_MODEL ** -0.5),
        'ff_w_down': nrm(ks[22], (N_EVEN, D_FF, D_MODEL), D_FF ** -0.5),
        'moe_w_router': nrm(ks[23], (N_ODD, D_MODEL, N_EXPERTS), D_MODEL ** -0.5),
        'moe_b_router': nrm(ks[24], (N_ODD, N_EXPERTS), 0.01),
        'moe_w_up': nrm(ks[25], (N_ODD, N_EXPERTS, D_MODEL, 2 * D_FF_EXPERT), D_MODEL ** -0.5),
        'moe_w_down': nrm(ks[26], (N_ODD, N_EXPERTS, D_FF_EXPERT, D_MODEL), D_FF_EXPERT ** -0.5),
        'final_norm_w': gain(ks[27], (D_MODEL,)),
    }


def reference(x_prompt, x_sample, state_hgrn, cache_k, cache_v, page_table, meta_tokens,
              norm_mix_w, norm_ffn_w, hg_w_in, hg_lower_bound, hg_norm_w, hg_w_out,
              at_w_qkv, at_lambda_q1, at_lambda_k1, at_lambda_q2, at_lambda_k2, at_subln_w,
              at_w_out, rel_bias_table, ff_w_up, ff_w_down, moe_w_router, moe_b_router,
              moe_w_up, moe_w_down, final_norm_w):
    B, T, _ = x_prompt.shape
    pad = LEAD - N_META
    xp = jnp.concatenate([jnp.zeros((B, pad, D_MODEL), x_prompt.dtype),
                          jnp.broadcast_to(meta_tokens.astype(x_prompt.dtype), (B, N_META, D_MODEL)),
                          x_prompt], axis=1)
    pos = jnp.arange(LEAD + T) - pad
    valid = pos >= 0
    xs = x_sample
    lower_bounds = jnp.cumsum(jax.nn.softmax(hg_lower_bound.astype(jnp.float32), axis=0), axis=0)
    st_p, st_s, k_p, v_p, k_s, v_s = [], [], [], [], [], []
    for layer in range(DEPTH):
        j = layer // 2
        hp = rmsnorm(xp, norm_mix_w[layer])
        hs = rmsnorm(xs, norm_mix_w[layer])
        if layer % 2 == 0:
            mp, sp = hgrn_prompt(hp, valid, hg_w_in[j], lower_bounds[j], hg_norm_w[j], hg_w_out[j])
            ms, ss = hgrn_sample(hs, state_hgrn[j], hg_w_in[j], lower_bounds[j], hg_norm_w[j], hg_w_out[j])
            st_p.append(sp)
            st_s.append(ss)
        else:
            lambda_init = 0.8 - 0.6 * math.exp(-0.3 * layer)
            lam = diff_lambda(at_lambda_q1[j], at_lambda_k1[j], at_lambda_q2[j], at_lambda_k2[j], lambda_init)
            mp, kp, vp = diff_attn_prompt(hp, pos, valid, at_w_qkv[j], at_w_out[j], lam, lambda_init,
                                          at_subln_w[j], rel_bias_table)
            ms, kn, vn = diff_attn_sample(hs, j, cache_k, cache_v, page_table, at_w_qkv[j], at_w_out[j],
                                          lam, lambda_init, at_subln_w[j], rel_bias_table)
            k_p.append(kp[:, pad:])
            v_p.append(vp[:, pad:])
            k_s.append(kn)
            v_s.append(vn)
        xp = xp + mp
        xs = xs + ms
        hp = rmsnorm(xp, norm_ffn_w[layer])
        hs = rmsnorm(xs, norm_ffn_w[layer])
        if layer % 2 == 0:
            xp = xp + swiglu(hp, ff_w_up[j], ff_w_down[j])
            xs = xs + swiglu(hs, ff_w_up[j], ff_w_down[j])
        else:
            xp = xp + moe_swiglu(hp, moe_w_router[j], moe_b_router[j], moe_w_up[j], moe_w_down[j])
            xs = xs + moe_swiglu(hs, moe_w_router[j], moe_b_router[j], moe_w_up[j], moe_w_down[j])
    y_prompt = rmsnorm(xp, final_norm_w)[:, LEAD:]
    y_sample = rmsnorm(xs, final_norm_w)
    return (y_prompt, y_sample, jnp.stack(st_p), jnp.stack(st_s),
            jnp.stack(k_p), jnp.stack(v_p), jnp.stack(k_s), jnp.stack(v_s))
```

```python
import os
from contextlib import ExitStack
import numpy as np
import ml_dtypes
import concourse.bass as bass
import concourse.mybir as mybir
from concourse.bass_utils import run_bass_kernel_spmd

F32 = mybir.dt.float32
BF16 = mybir.dt.bfloat16
I32 = mybir.dt.int32
ALU = mybir.AluOpType
AF = mybir.ActivationFunctionType
AX = mybir.AxisListType

NCORES = 8
D = 1024
NT = 17
TAIL = 16
NTOK = 2048 + 80
EPS = 1e-6
N_PHYS = 2560
DFF = 2816
DFFE = 3584
SEM_LIMIT = 30000


class Buf:
    __slots__ = ("name", "w", "r", "dsem", "dcnt", "strict")

    def __init__(self, name, strict=False):
        self.name = name
        self.strict = strict
        self.w = None
        self.r = []
        self.dsem = None
        self.dcnt = 0


class Eng:
    def __init__(self, name, h):
        self.name = name
        self.h = h
        self.sem = None
        self.cnt = 0
        self.seen = {}
        self.pend_r = []
        self.pend_w = []
        self.n_inst = 0
        self.n_wait = 0


class Sched:
    def __init__(self, nc, stack):
        self.nc = nc
        self.stack = stack
        self.nsem = 0
        self.E = {}
        self.dbufs = []
        self.old_sems = []
        self.force_same = False
        for name in ("tensor", "vector", "scalar", "gpsimd", "sync"):
            e = Eng(name, getattr(nc, name))
            self.E[name] = e
            self._new_sem(e)

    def _alloc_sem(self, name):
        self.nsem += 1
        return self.stack.enter_context(self.nc.semaphore(f"{name}_{self.nsem}"))

    def _new_sem(self, e):
        if e.sem is not None:
            self.old_sems.append((e.sem, e.cnt, e.name))
        e.sem = self._alloc_sem("e_" + e.name)
        e.cnt = 0

    def _wait(self, e, tok, strict=False):
        sem, val, owner = tok
        if owner == e.name and not (strict or self.force_same):
            return
        k = id(sem)
        if e.seen.get(k, 0) >= val:
            return
        e.h.wait_ge(sem, val)
        e.seen[k] = val
        e.n_wait += 1

    def _deps(self, e, reads, writes):
        for b in reads:
            if b.w is not None:
                self._wait(e, b.w, b.strict)
        for b in writes:
            if b.w is not None:
                self._wait(e, b.w, b.strict)
            for t in b.r:
                self._wait(e, t)

    @staticmethod
    def _compact(toks):
        best = {}
        for t in toks:
            k = id(t[0])
            if k not in best or best[k][1] < t[1]:
                best[k] = t
        return list(best.values())

    def op(self, eng, fn, reads=(), writes=(), inc=True):
        e = self.E[eng]
        self._deps(e, reads, writes)
        inst = fn(e.h)
        e.n_inst += 1
        e.pend_r.extend(reads)
        e.pend_w.extend(writes)
        if inc:
            if e.cnt >= SEM_LIMIT:
                self._new_sem(e)
            e.cnt += 1
            inst.then_inc(e.sem, 1)
            tok = (e.sem, e.cnt, e.name)
            for b in e.pend_w:
                b.w = tok
                b.r = []
            for b in e.pend_r:
                if b.w is not tok:
                    b.r.append(tok)
                    if len(b.r) > 10:
                        b.r = self._compact(b.r)
            e.pend_r = []
            e.pend_w = []
        return inst

    def dma(self, q, fn, reads=(), writes=(), sembuf=None):
        e = self.E[q]
        self._deps(e, reads, writes)
        sb = sembuf or (writes[0] if writes else reads[0])
        if sb.dsem is None or sb.dcnt + 16 > 60000:
            sb.dsem = self._alloc_sem("d_" + sb.name)
            sb.dcnt = 0
            if sb not in self.dbufs:
                self.dbufs.append(sb)
        inst = fn(e.h)
        e.n_inst += 1
        sb.dcnt += 16
        inst.then_inc(sb.dsem, 16)
        tok = (sb.dsem, sb.dcnt, None)
        for b in writes:
            b.w = tok
            b.r = []
        for b in reads:
            b.r.append(tok)
            if len(b.r) > 10:
                b.r = self._compact(b.r)
        return inst

    def wait_all(self, eng, bufs):
        e = self.E[eng]
        for b in bufs:
            if b.w is not None:
                self._wait(e, b.w)
            for t in b.r:
                self._wait(e, t)

    def barrier(self):
        toks = [(e.sem, e.cnt, e.name) for e in self.E.values() if e.cnt > 0]
        toks += [(b.dsem, b.dcnt, None) for b in self.dbufs if b.dcnt > 0]
        for e in self.E.values():
            for t in toks:
                self._wait(e, t)


def _bucket(n):
    n = max(int(n), 0)
    if n < 16:
        return n
    nf = np.float32(max(n, 1))
    large = 16 + int(np.float32(np.float32(np.log(np.float32(nf / np.float32(16)))) / np.float32(np.log(8.0))) * np.float32(16))
    return min(large, 31)


def host_consts():
    c = {}
    c["ident_f"] = np.eye(128, dtype=np.float32)
    m = (np.arange(64)[:, None] <= np.arange(64)[None, :]).astype(np.float32)
    c["hmask"] = np.tile(np.tile(m, (1, 8)), (2, 1))
    tm = np.zeros((80, 80), np.float32)
    for s in range(64):
        for t in range(64):
            if s // 4 == t // 4 and s <= t:
                tm[s, t] = 1
    for s in range(64, 80):
        for t in range(64, 80):
            if s <= t:
                tm[s, t] = 1
    tmf = np.zeros((128, 8 * 80), np.float32)
    tmf[:80] = np.tile(tm, (1, 8))
    c["tmask"] = tmf
    rp = np.ones((128, 256), np.float32)
    rp[:, ::64] = 0
    c["reset_p"] = rp
    rt = np.ones((128, 80), np.float32)
    rt[:, 0:64:4] = 0
    rt[:, 64] = 0
    c["reset_t"] = rt
    rm = np.zeros((64, 16), np.float32)
    for b in range(16):
        rm[4 * b:4 * b + 4, b] = 1
    c["rowmask"] = rm
    oh = np.zeros((33, 384), np.float32)
    for i in range(384):
        dist = 255 - i
        if dist < 0:
            oh[32, i] = 1.0
        else:
            oh[_bucket(dist), i] += 1.0
            oh[31, i] -= 1.0
    c["oh"] = oh
    c["jflip"] = np.ascontiguousarray(np.eye(128, dtype=np.float32)[::-1])
    c["piota"] = np.arange(128, dtype=np.float32).reshape(128, 1)
    return c


CONST_SHAPES = {"ident_f": [128, 128], "hmask": [128, 512], "tmask": [128, 640], "reset_p": [128, 256],
                "reset_t": [128, 80], "rowmask": [64, 16], "oh": [33, 384], "jflip": [128, 128], "piota": [128, 1]}

WEIGHT_SHAPES = {
    "meta_tokens": [16, D], "norm_mix_w": [2, D], "norm_ffn_w": [2, D], "hg_w_in": [D, 4 * D],
    "hg_lower_bound": [2, D], "hg_norm_w": [1, D], "hg_w_out": [D, D], "at_w_qkv": [D, 3 * D],
    "at_lambda_q1": [1, 64], "at_lambda_k1": [1, 64], "at_lambda_q2": [1, 64], "at_lambda_k2": [1, 64],
    "at_subln_w": [1, 128], "at_w_out": [D, D], "rel_bias_table": [32, 8], "ff_w_up": [D, 2 * DFF],
    "ff_w_down": [DFF, D], "moe_w_router": [D, 8], "moe_b_router": [1, 8], "moe_w_up": [8 * D, 2 * DFFE],
    "moe_w_down": [8 * DFFE, D], "final_norm_w": [1, D],
}


def build(stage=99, dbg=False, nphys=N_PHYS, att_stop=0, do_sample=True, skip0=False, var=0):
    nc = bass.Bass("TRN2", target_bir_lowering=False)
    DUMPS = {}
    I = {}
    I["xp"] = nc.dram_tensor("xp", [2048, D], F32, kind="ExternalInput").ap()
    I["xs"] = nc.dram_tensor("xs", [64, D], F32, kind="ExternalInput").ap()
    I["state"] = nc.dram_tensor("state", [16, 8, 128, 128], F32, kind="ExternalInput").ap()
    I["cache_k"] = nc.dram_tensor("cache_k", [nphys * 128, D], F32, kind="ExternalInput").ap()
    I["cache_v"] = nc.dram_tensor("cache_v", [nphys * 128, D], F32, kind="ExternalInput").ap()
    I["ptab"] = nc.dram_tensor("ptab", [16, 16], I32, kind="ExternalInput").ap()
    for k, s in WEIGHT_SHAPES.items():
        I[k] = nc.dram_tensor(k, s, F32, kind="ExternalInput").ap()
    for k, s in CONST_SHAPES.items():
        I[k] = nc.dram_tensor(k, s, F32, kind="ExternalInput").ap()
    O = {}
    O["y_p"] = nc.dram_tensor("y_p", [2048, D], F32, kind="ExternalOutput").ap()
    O["y_s"] = nc.dram_tensor("y_s", [64, D], F32, kind="ExternalOutput").ap()
    O["st_p"] = nc.dram_tensor("st_p", [8, 128, 128], F32, kind="ExternalOutput").ap()
    O["st_s"] = nc.dram_tensor("st_s", [16, 8, 128, 128], F32, kind="ExternalOutput").ap()
    O["k_p"] = nc.dram_tensor("k_p", [2064, D], F32, kind="ExternalOutput").ap()
    O["v_p"] = nc.dram_tensor("v_p", [2064, D], F32, kind="ExternalOutput").ap()
    O["k_s"] = nc.dram_tensor("k_s", [64, D], F32, kind="ExternalOutput").ap()
    O["v_s"] = nc.dram_tensor("v_s", [64, D], F32, kind="ExternalOutput").ap()
    if dbg:
        O["dbg_x"] = nc.dram_tensor("dbg_x", [NT * 128, D], F32, kind="ExternalOutput").ap()
    obuf = Buf("dram_out")

    with ExitStack() as st:
        st.enter_context(nc.allow_low_precision(reason="bf16 matmul operands, fp32 accumulation"))
        S = Sched(nc, st)

        sbctr = [0]

        def sb(stack, name, shape, dt):
            sbctr[0] += 1
            return stack.enter_context(nc.sbuf_tensor(f"s{sbctr[0]}_{name}", shape, dt))

        X = sb(st, "X", [128, NT, D], F32)
        bX = [Buf(f"X{t}") for t in range(NT)]
        ident_f = sb(st, "ident_f", [128, 128], F32); b_idf = Buf("ident_f")
        ident_b = sb(st, "ident_b", [128, 128], BF16); b_idb = Buf("ident_b")
        NSLOT = 4
        ring = [sb(st, f"ring{i}", [128, 4096], BF16) for i in range(NSLOT)]
        bring = [Buf(f"ring{i}") for i in range(NSLOT)]
        ring_i = [0]
        PS = [st.enter_context(nc.psum_tensor(f"ps{i}", [128, 512], F32)) for i in range(7)]
        bPS = [Buf(f"ps{i}") for i in range(7)]
        PSB = st.enter_context(nc.psum_tensor("psb", [128, 1024], BF16)); bPSB = Buf("psb")
        stat = sb(st, "stat", [128, 64], F32)
        bstat = Buf("stat", True)
        junk = sb(st, "junk", [128, D], F32); bjunk = Buf("junk")
        epsc = sb(st, "epsc", [128, 2], F32); b_epsc = Buf("epsc", True)
        wrow = sb(st, "wrow", [128, 2, D], F32)
        b_wrow = [Buf(f"wrow{i}") for i in range(2)]
        WR = {}
        WSRC = {"mix0": I["norm_mix_w"][0:1, :], "mix1": I["norm_mix_w"][1:2, :], "ffn0": I["norm_ffn_w"][0:1, :],
                "ffn1": I["norm_ffn_w"][1:2, :], "hgn": I["hg_norm_w"][0:1, :], "fin": I["final_norm_w"][0:1, :]}

        def load_wrow(nm, i):
            WR[nm] = i
            S.dma("sync", lambda h: h.dma_start(out=wrow[:, i, :], in_=WSRC[nm].partition_broadcast(128)), writes=[b_wrow[i]])

        def v_(f, reads=(), writes=(), inc=True):
            return S.op("vector", f, reads, writes, inc)

        def a_(f, reads=(), writes=(), inc=True):
            return S.op("scalar", f, reads, writes, inc)

        def p_(f, reads=(), writes=(), inc=True):
            return S.op("gpsimd", f, reads, writes, inc)

        def t_(f, reads=(), writes=(), inc=True):
            return S.op("tensor", f, reads, writes, inc)

        def dump(name, ap, shape, buf, dt=F32):
            if not dbg or name in DUMPS:
                return
            DUMPS[name] = nc.dram_tensor("dmp_" + name, shape, dt, kind="ExternalOutput").ap()
            S.dma("sync", lambda h: h.dma_start(out=DUMPS[name], in_=ap), reads=buf, writes=[obuf], sembuf=buf[0])

        v_(lambda h: h.memset(epsc[:, 0:1], EPS), [], [b_epsc])
        S.dma("sync", lambda h: h.dma_start(out=ident_f[:], in_=I["ident_f"][:, :]), writes=[b_idf])
        v_(lambda h: h.tensor_copy(ident_b[:], ident_f[:]), [b_idf], [b_idb])
        for t in range(16):
            S.dma("sync", lambda h: h.dma_start(out=X[:, t, :], in_=I["xp"][t * 128:(t + 1) * 128, :]), writes=[bX[t]])
        S.dma("sync", lambda h: h.dma_start(out=X[0:64, TAIL, :], in_=I["xs"][:, :]), writes=[bX[TAIL]])
        S.dma("sync", lambda h: h.dma_start(out=X[64:80, TAIL, :], in_=I["meta_tokens"][:, :]), writes=[bX[TAIL]])
        load_wrow("mix0", 0)
        load_wrow("hgn", 1)

        def rows(t):
            return 80 if t == TAIL else 128

        def wload(src_ap, nk, ncols, q="gpsimd"):
            i = ring_i[0] % NSLOT
            ring_i[0] += 1
            assert nk * ncols <= 4096
            if nk == 8:
                view = ring[i][:, :].rearrange("p (k n) -> p k n", k=8)[:, :, 0:ncols]
            else:
                view = ring[i][:, 0:nk * ncols].rearrange("p (k n) -> p k n", k=nk)
            S.dma(q, lambda h: h.dma_start(out=view, in_=src_ap.rearrange("(k p) n -> p k n", p=128)), writes=[bring[i]])
            return view, bring[i]

        def rmsnorm_to_hT(t, wname, hT_dst, b_hT, extra_fp32=None):
            r = rows(t)
            c0 = (t % 16)
            ss = stat[:r, 0:1]
            a_(lambda h: h.activation(junk[:r, :], X[:r, t, :], AF.Square, accum_out=ss), [bX[t]], [bjunk, bstat])
            a_(lambda h: h.activation(stat[:r, 2:3], ss, AF.Ln, scale=1.0 / D, bias=epsc[:r, 0:1]), [bstat, b_epsc], [bstat])
            a_(lambda h: h.activation(stat[:r, 3:4], stat[:r, 2:3], AF.Exp, scale=-0.5), [bstat], [bstat])
            hb = hbf[:r, :]
            v_(lambda h: h.scalar_tensor_tensor(hb, X[:r, t, :], stat[:r, 3:4], wrow[:r, WR[wname], :], ALU.mult, ALU.mult),
               [bX[t], bstat, b_wrow[WR[wname]]], [b_hbf])
            for k in range(8):
                t_(lambda h: h.transpose(PSB[:, k * 128:k * 128 + r], hbf[:r, k * 128:(k + 1) * 128], ident_b[:r, :r]),
                   [b_hbf, b_idb], [bPSB], inc=(k == 7))
            src = PSB[:, :].rearrange("p (k n) -> p k n", k=8)[:, :, 0:r]
            a_(lambda h: h.copy(hT_dst, src), [bPSB], [b_hT])

        hbf = sb(st, "hbf", [128, D], BF16); b_hbf = Buf("hbf")

        SEG = 2
        NSEGC = 4
        with ExitStack() as ph:
          if not skip0:
              lbt = sb(ph, "lbt", [128, 8, 8], F32); b_lbt = Buf("lbt", True)
              S.dma("sync", lambda h: h.dma_start(out=lbt[:, 0, :], in_=I["hg_lower_bound"][0:1, :].rearrange("o (k p) -> p (o k)", p=128), allow_slow_non_contiguous=True), writes=[b_lbt])
              S.dma("sync", lambda h: h.dma_start(out=lbt[:, 1, :], in_=I["hg_lower_bound"][1:2, :].rearrange("o (k p) -> p (o k)", p=128), allow_slow_non_contiguous=True), writes=[b_lbt])
              v_(lambda h: h.tensor_tensor(lbt[:, 2, :], lbt[:, 1, :], lbt[:, 0, :], ALU.subtract), [b_lbt], [b_lbt])
              a_(lambda h: h.activation(lbt[:, 3, :], lbt[:, 2, :], AF.Exp), [b_lbt], [b_lbt])
              v_(lambda h: h.tensor_scalar(lbt[:, 3, :], lbt[:, 3, :], 1.0, None, ALU.add), [b_lbt], [b_lbt])
              v_(lambda h: h.reciprocal(lbt[:, 4, :], lbt[:, 3, :]), [b_lbt], [b_lbt])
              v_(lambda h: h.tensor_scalar(lbt[:, 5, :], lbt[:, 4, :], -1.0, 1.0, ALU.mult, ALU.add), [b_lbt], [b_lbt])
              LB = lambda hd: lbt[:, 4, hd:hd + 1]
              OML = lambda hd: lbt[:, 5, hd:hd + 1]

              hmask = sb(ph, "hmask", [128, 512], F32); b_hmask = Buf("hmask")
              tmask = sb(ph, "tmask", [128, 640], F32); b_tmask = Buf("tmask")
              reset_p = sb(ph, "reset_p", [128, 256], F32); b_rp = Buf("reset_p")
              reset_t = sb(ph, "reset_t", [128, 80], F32); b_rt = Buf("reset_t")
              rowmask = sb(ph, "rowmask", [64, 16], F32); b_rm = Buf("rowmask")
              for tl, bf, nm in ((hmask, b_hmask, "hmask"), (tmask, b_tmask, "tmask"), (reset_p, b_rp, "reset_p"),
                                 (reset_t, b_rt, "reset_t"), (rowmask, b_rm, "rowmask")):
                  S.dma("sync", lambda h: h.dma_start(out=tl[:], in_=I[nm][:, :]), writes=[bf])

              NS = SEG * 128
              hT = sb(ph, "hT", [128, 8, NS], BF16); b_hT = Buf("hT")
              qT = sb(ph, "qT", [128, 8, NS], BF16); b_qT = [Buf(f"qT{h}") for h in range(8)]
              kT = sb(ph, "kT", [128, 8, NS], BF16); b_kT = [Buf(f"kT{h}") for h in range(8)]
              vtok = sb(ph, "vtok", [128, SEG, D], BF16); b_vtok = [Buf(f"vtok{i}") for i in range(SEG)]
              sgt = sb(ph, "sgt", [128, SEG, D], BF16); b_sgt = [Buf(f"sgt{i}") for i in range(SEG)]
              ktok = sb(ph, "ktok", [128, SEG, D], BF16); b_ktok = [Buf(f"ktok{i}") for i in range(SEG)]
              f1 = sb(ph, "f1", [128, 512], F32); b_f1 = Buf("f1")
              f2 = sb(ph, "f2", [128, 512], F32); b_f2 = Buf("f2")
              f3 = sb(ph, "f3", [128, 512], F32); b_f3 = Buf("f3")
              Gt = sb(ph, "Gt", [128, NS], F32); b_G = Buf("Gt")
              csc = sb(ph, "csc", [128, 8, 8, 4], F32); b_csc = Buf("csc", True)
              Sst = sb(ph, "Sst", [128, 8, 128], F32); b_S = Buf("Sst")
              Sp = sb(ph, "Sp", [128, 8, 128], BF16); b_Sp = Buf("Sp")
              AT = sb(ph, "AT", [128, 8, 80], BF16); b_AT = Buf("AT")
              og = sb(ph, "og", [128, D], F32); b_og = Buf("og")
              ogb = sb(ph, "ogb", [128, D], BF16); b_ogb = Buf("ogb")
              ogT = sb(ph, "ogT", [128, 8, NS], BF16); b_ogT = [Buf(f"ogT{i}") for i in range(SEG)]
              v_(lambda h: h.memset(Sst[:], 0.0), [], [b_S])
              v_(lambda h: h.memset(Sp[:], 0.0), [], [b_Sp])

              W = I["hg_w_in"]

              def hgrn_segment(seg):
                  tail = seg == "tail"
                  tiles = [TAIL] if tail else [SEG * seg + i for i in range(SEG)]
                  N = 80 if tail else NS
                  for i, t in enumerate(tiles):
                      r = rows(t)
                      rmsnorm_to_hT(t, "mix0", hT[:, :, i * 128:i * 128 + r], b_hT)
                  for half in range(2):
                      wq, bwq = wload(W[:, half * 512:(half + 1) * 512], 8, 512)
                      wf, bwf = wload(W[:, 1024 + half * 512:1024 + (half + 1) * 512], 8, 512)
                      for hh in range(4):
                          hd = half * 4 + hh
                          pq, bpq, pf, bpf = PS[0], bPS[0], PS[1], bPS[1]
                          for k in range(8):
                              t_(lambda h: h.matmul(pq[:, :N], wq[:, k, hh * 128:(hh + 1) * 128], hT[:, k, :N], start=(k == 0), stop=(k == 7)),
                                 [bwq, b_hT], [bpq], inc=(k == 7))
                          for k in range(8):
                              t_(lambda h: h.matmul(pf[:, :N], wf[:, k, hh * 128:(hh + 1) * 128], hT[:, k, :N], start=(k == 0), stop=(k == 7)),
                                 [bwf, b_hT], [bpf], inc=(k == 7))
                          a_(lambda h: h.activation(f1[:, :N], pq[:, :N], AF.Exp, scale=-1.0), [bpq], [b_f1])
                          v_(lambda h: h.tensor_scalar(f1[:, :N], f1[:, :N], 1.0, None, ALU.add), [b_f1], [b_f1])
                          v_(lambda h: h.reciprocal(f1[:, :N], f1[:, :N]), [b_f1], [b_f1])
                          v_(lambda h: h.tensor_tensor(f1[:, :N], pq[:, :N], f1[:, :N], ALU.mult), [bpq, b_f1], [b_f1])
                          a_(lambda h: h.activation(f2[:, :N], pf[:, :N], AF.Exp, scale=-1.0), [bpf], [b_f2])
                          v_(lambda h: h.tensor_scalar(f2[:, :N], f2[:, :N], 1.0, None, ALU.add), [b_f2], [b_f2])
                          v_(lambda h: h.reciprocal(f2[:, :N], f2[:, :N]), [b_f2], [b_f2])
                          v_(lambda h: h.tensor_scalar(f2[:, :N], f2[:, :N], OML(hd), LB(hd), ALU.mult, ALU.add), [b_f2, b_lbt], [b_f2])
                          a_(lambda h: h.activation(f3[:, :N], f2[:, :N], AF.Ln), [b_f2], [b_f3])
                          v_(lambda h: h.tensor_scalar(f2[:, :N], f2[:, :N], -1.0, 1.0, ALU.mult, ALU.add), [b_f2], [b_f2])
                          rs = reset_t if tail else reset_p
                          v_(lambda h: h.tensor_tensor_scan(Gt[:, :N], rs[:, :N], f3[:, :N], 0.0, ALU.mult, ALU.add),
                             [b_f3, b_rt if tail else b_rp], [b_G])
                          if not tail:
                              G3 = Gt[:, :].rearrange("p (c n) -> p c n", n=64)
                              a_(lambda h: h.activation(csc[:, hd, 0:NSEGC, 0], G3[:, :, 31], AF.Exp), [b_G], [b_csc])
                              a_(lambda h: h.activation(csc[:, hd, 0:NSEGC, 1], G3[:, :, 63], AF.Exp), [b_G], [b_csc])
                              v_(lambda h: h.tensor_tensor(csc[:, hd, 0:NSEGC, 3], G3[:, :, 63], G3[:, :, 31], ALU.subtract), [b_G], [b_csc])
                              a_(lambda h: h.activation(csc[:, hd, 0:NSEGC, 2], csc[:, hd, 0:NSEGC, 3], AF.Exp), [b_csc], [b_csc])
                              f33 = f3[:, 0:NS].rearrange("p (c n) -> p c n", n=64)
                              v_(lambda h: h.tensor_tensor(f33, G3, G3[:, :, 31:32].to_broadcast([128, NSEGC, 64]), ALU.subtract), [b_G], [b_f3])
                              gc, b_gc = f3, b_f3
                          else:
                              cflat = csc[:, hd, :, :].rearrange("p a b -> p (a b)")
                              a_(lambda h: h.activation(cflat[:, 0:16], Gt[:, 3:64:4], AF.Exp), [b_G], [b_csc])
                              a_(lambda h: h.activation(cflat[:, 16:17], Gt[:, 79:80], AF.Exp), [b_G], [b_csc])
                              gc, b_gc = Gt, b_G
                          a_(lambda h: h.activation(junk[:, :N], gc[:, :N], AF.Exp), [b_gc], [bjunk])
                          v_(lambda h: h.tensor_tensor(qT[:, hd, :N], f1[:, :N], junk[:, :N], ALU.mult), [b_f1, bjunk], [b_qT[hd]])
                          a_(lambda h: h.activation(junk[:, 512:512 + N], gc[:, :N], AF.Exp, scale=-1.0), [b_gc], [bjunk])
                          v_(lambda h: h.tensor_tensor(kT[:, hd, :N], f2[:, :N], junk[:, 512:512 + N], ALU.mult), [b_f2, bjunk], [b_kT[hd]])
                  for half in range(2):
                      wi, bwi = wload(W[:, 2048 + half * 512:2048 + (half + 1) * 512], 8, 512)
                      wg, bwg = wload(W[:, 3072 + half * 512:3072 + (half + 1) * 512], 8, 512)
                      for i, t in enumerate(tiles):
                          r = rows(t)
                          pv, bpv, pg, bpg = PS[2], bPS[2], PS[3], bPS[3]
                          for k in range(8):
                              t_(lambda h: h.matmul(pv[:r, :], hT[:, k, i * 128:i * 128 + r], wi[:, k, :], start=(k == 0), stop=(k == 7)),
                                 [bwi, b_hT], [bpv], inc=(k == 7))
                          for k in range(8):
                              t_(lambda h: h.matmul(pg[:r, :], hT[:, k, i * 128:i * 128 + r], wg[:, k, :], start=(k == 0), stop=(k == 7)),
                                 [bwg, b_hT], [bpg], inc=(k == 7))
                          a_(lambda h: h.copy(vtok[:r, i, half * 512:(half + 1) * 512], pv[:r, :]), [bpv], [b_vtok[i]])
                          a_(lambda h: h.activation(f3[:r, :], pg[:r, :], AF.Exp, scale=-1.0), [bpg], [b_f3])
                          v_(lambda h: h.tensor_scalar(f3[:r, :], f3[:r, :], 1.0, None, ALU.add), [b_f3], [b_f3])
                          v_(lambda h: h.reciprocal(sgt[:r, i, half * 512:(half + 1) * 512], f3[:r, :]), [b_f3], [b_sgt[i]])
                  for i, t in enumerate(tiles):
                      r = rows(t)
                      for hd in range(8):
                          t_(lambda h: h.transpose(PSB[:r, hd * 128:(hd + 1) * 128], kT[:, hd, i * 128:i * 128 + r], ident_b[:, :]),
                             [b_kT[hd], b_idb], [bPSB], inc=(hd == 7))
                      a_(lambda h: h.copy(ktok[:r, i, :], PSB[:r, :]), [bPSB], [b_ktok[i]])
                  tg = "t" if tail else f"s{seg}"
                  if tail or seg == 0:
                      dump(f"hT_{tg}", hT[:, :, :], [128, 8, NS], [b_hT], BF16)
                      dump(f"qT_{tg}", qT[:, :, :], [128, 8, NS], b_qT, BF16)
                      dump(f"kT_{tg}", kT[:, :, :], [128, 8, NS], b_kT, BF16)
                      dump(f"vtok_{tg}", vtok[:, :, :], [128, SEG, D], b_vtok, BF16)
                      dump(f"sgt_{tg}", sgt[:, :, :], [128, SEG, D], b_sgt, BF16)
                      dump(f"ktok_{tg}", ktok[:, :, :], [128, SEG, D], b_ktok, BF16)
                      dump(f"csc_{tg}", csc[:, :, :, :], [128, 8, 8, 4], [b_csc])
                      dump(f"G_{tg}", Gt[:, :], [128, NS], [b_G])
                  return tiles

              def gate_norm(t, i, po_list):
                  r = rows(t)
                  for hf in range(2):
                      v_(lambda h: h.tensor_tensor(og[:r, hf * 512:(hf + 1) * 512], po_list[hf][0][:r, :], sgt[:r, i, hf * 512:(hf + 1) * 512], ALU.mult),
                         [po_list[hf][1], b_sgt[i]], [b_og])
                  ss = stat[:r, 8:9]
                  dump(f"og_{t}", og[:, :], [128, D], [b_og])
                  a_(lambda h: h.activation(junk[:r, :], og[:r, :], AF.Square, accum_out=ss), [b_og], [bjunk, bstat])
                  a_(lambda h: h.activation(stat[:r, 10:11], ss, AF.Ln, scale=1.0 / D, bias=epsc[:r, 0:1]), [bstat, b_epsc], [bstat])
                  a_(lambda h: h.activation(stat[:r, 11:12], stat[:r, 10:11], AF.Exp, scale=-0.5), [bstat], [bstat])
                  v_(lambda h: h.scalar_tensor_tensor(ogb[:r, :], og[:r, :], stat[:r, 11:12], wrow[:r, WR["hgn"], :], ALU.mult, ALU.mult),
                     [b_og, bstat, b_wrow[WR["hgn"]]], [b_ogb])
                  for k in range(8):
                      t_(lambda h: h.transpose(PSB[:, k * 128:k * 128 + r], ogb[:r, k * 128:(k + 1) * 128], ident_b[:r, :r]),
                         [b_ogb, b_idb], [bPSB], inc=(k == 7))
                  a_(lambda h: h.copy(ogT[:, :, i * 128:i * 128 + r], PSB[:, :].rearrange("p (k n) -> p k n", k=8)[:, :, 0:r]), [bPSB], [b_ogT[i]])

              def out_proj(tiles):
                  for hf in range(2):
                      wo, bwo = wload(I["hg_w_out"][:, hf * 512:(hf + 1) * 512], 8, 512)
                      for i, t in enumerate(tiles):
                          r = rows(t)
                          pz, bpz = PS[4 + (i % 2)], bPS[4 + (i % 2)]
                          for k in range(8):
                              t_(lambda h: h.matmul(pz[:r, :], ogT[:, k, i * 128:i * 128 + r], wo[:, k, :], start=(k == 0), stop=(k == 7)),
                                 [b_ogT[i], bwo], [bpz], inc=(k == 7))
                          v_(lambda h: h.tensor_tensor(X[:r, t, hf * 512:(hf + 1) * 512], X[:r, t, hf * 512:(hf + 1) * 512], pz[:r, :], ALU.add),
                             [bX[t], bpz], [bX[t]])

              S.force_same = True
              hgrn_segment("tail")
              with ExitStack() as tl:
                  S0b = sb(tl, "S0b", [128, 2, 16, 128], BF16); b_S0b = [Buf("S0b0"), Buf("S0b1")]
                  Z = sb(tl, "Z", [128, 2, 16, 80], BF16); b_Z = [Buf("Z0"), Buf("Z1")]
                  KM = sb(tl, "KM", [64, D], BF16); b_KM = Buf("KM")
                  S0f = sb(tl, "S0f", [128, 1, D], F32); b_S0f = [Buf("S0f0")]
                  Sn = og[:, :].rearrange("p (o n) -> p o n", o=1); b_Sn = [b_og]
                  v_(lambda h: h.memset(Z[:], 0.0), [], b_Z)
                  for hd in range(8):
                      pa, bpa = (PS[0], bPS[0]) if hd < 4 else (PS[1], bPS[1])
                      t_(lambda h: h.matmul(pa[:80, (hd % 4) * 80:(hd % 4) * 80 + 80], kT[:, hd, 0:80], qT[:, hd, 0:80], start=True, stop=True),
                         [b_kT[hd], b_qT[hd]], [bpa], inc=(hd % 4 == 3))
                  v_(lambda h: h.tensor_tensor(AT[:80, 0:4, :], PS[0][:80, 0:320].rearrange("p (a b) -> p a b", a=4),
                                               tmask[:80, 0:320].rearrange("p (a b) -> p a b", a=4), ALU.mult), [bPS[0], b_tmask], [b_AT])
                  v_(lambda h: h.tensor_tensor(AT[:80, 4:8, :], PS[1][:80, 0:320].rearrange("p (a b) -> p a b", a=4),
                                               tmask[:80, 320:640].rearrange("p (a b) -> p a b", a=4), ALU.mult), [bPS[1], b_tmask], [b_AT])
                  for hd in range(8):
                      j = hd % 2
                      S.dma("gpsimd", lambda h: h.dma_start(out=S0b[:, j, :, :], in_=I["state"][:, hd, :, :].rearrange("b p v -> p b v")), writes=[b_S0b[j]])
                      for b in range(16):
                          v_(lambda h: h.tensor_copy(Z[:, j, b, 4 * b:4 * b + 4], qT[:, hd, 4 * b:4 * b + 4]), [b_qT[hd]], [b_Z[j]])
                      po, bpo = (PS[2], bPS[2]) if hd < 4 else (PS[3], bPS[3])
                      c0 = (hd % 4) * 128
                      t_(lambda h: h.matmul(po[:80, c0:c0 + 128], AT[:80, hd, :], vtok[:80, 0, hd * 128:(hd + 1) * 128], start=True, stop=False),
                         [b_AT, b_vtok[0]], [bpo], inc=False)
                      for b in range(16):
                          t_(lambda h: h.matmul(po[:80, c0:c0 + 128], Z[:, j, b, :], S0b[:, j, b, :], start=False, stop=(b == 15)),
                             [b_Z[j], b_S0b[j]], [bpo], inc=(b == 15))
                  gate_norm(TAIL, 0, [(PS[2], bPS[2]), (PS[3], bPS[3])])
                  out_proj([TAIL])
                  cflat = lambda hd: csc[:, hd, :, :].rearrange("p a b -> p (a b)")
                  for b in range(16):
                      j = 0
                      S.dma("sync", lambda h: h.dma_start(out=S0f[:, j, :].rearrange("p (hh v) -> p hh v", hh=8),
                                                          in_=I["state"][b].rearrange("hh p v -> p hh v")), writes=[b_S0f[j]])
                      v_(lambda h: h.tensor_scalar(KM[:, :], ktok[0:64, 0, :], rowmask[:, b:b + 1], None, ALU.mult), [b_ktok[0], b_rm], [b_KM])
                      for hd in range(8):
                          pm, bpm = (PS[4], bPS[4]) if hd < 4 else (PS[5], bPS[5])
                          c0 = (hd % 4) * 128
                          t_(lambda h: h.matmul(pm[:, c0:c0 + 128], KM[:, hd * 128:(hd + 1) * 128], vtok[0:64, 0, hd * 128:(hd + 1) * 128], start=True, stop=True),
                             [b_KM, b_vtok[0]], [bpm], inc=(hd % 4 == 3))
                      for hd in range(8):
                          pm, bpm = (PS[4], bPS[4]) if hd < 4 else (PS[5], bPS[5])
                          c0 = (hd % 4) * 128
                          v_(lambda h: h.tensor_tensor(Sn[:, j, hd * 128:(hd + 1) * 128], pm[:, c0:c0 + 128], S0f[:, j, hd * 128:(hd + 1) * 128], ALU.add),
                             [bpm, b_S0f[j]], [b_Sn[j]])
                          v_(lambda h: h.tensor_scalar(Sn[:, j, hd * 128:(hd + 1) * 128], Sn[:, j, hd * 128:(hd + 1) * 128], cflat(hd)[:, b:b + 1], None, ALU.mult),
                             [b_csc], [b_Sn[j]])
                      S.dma("sync", lambda h: h.dma_start(out=O["st_s"][b].rearrange("hh p v -> p hh v"),
                                                          in_=Sn[:, j, :].rearrange("p (hh v) -> p hh v", hh=8)), reads=[b_Sn[j]], writes=[obuf], sembuf=b_Sn[j])
                  for hd in range(8):
                      pm, bpm = (PS[4], bPS[4]) if hd < 4 else (PS[5], bPS[5])
                      c0 = (hd % 4) * 128
                      t_(lambda h: h.matmul(pm[:, c0:c0 + 128], ktok[64:80, 0, hd * 128:(hd + 1) * 128], vtok[64:80, 0, hd * 128:(hd + 1) * 128], start=True, stop=True),
                         [b_ktok[0], b_vtok[0]], [bpm], inc=(hd % 4 == 3))
                  for hd in range(8):
                      pm, bpm = (PS[4], bPS[4]) if hd < 4 else (PS[5], bPS[5])
                      c0 = (hd % 4) * 128
                      v_(lambda h: h.tensor_scalar(Sst[:, hd, :], pm[:, c0:c0 + 128], cflat(hd)[:, 16:17], None, ALU.mult), [bpm, b_csc], [b_S])
                  S.barrier()
              S.force_same = False
              for seg in range(16 // SEG):
                  tiles = hgrn_segment(seg)
                  for ci in range(NSEGC):
                      i = ci // 2
                      t = tiles[i]
                      p0 = 64 * (ci % 2)
                      cs = slice(ci * 64, ci * 64 + 64)
                      for hd in range(8):
                          v_(lambda h: h.tensor_scalar(Sp[:, hd, :], Sst[:, hd, :], csc[:, hd, ci, 0:1], None, ALU.mult), [b_S, b_csc], [b_Sp])
                      pa, bpa = PS[0], bPS[0]
                      for hd in range(8):
                          t_(lambda h: h.matmul(pa[p0:p0 + 64, hd * 64:(hd + 1) * 64], kT[:, hd, cs], qT[:, hd, cs], start=True, stop=True),
                             [b_kT[hd], b_qT[hd]], [bpa], inc=(hd == 7))
                      v_(lambda h: h.tensor_tensor(AT[p0:p0 + 64, :, 0:64], pa[p0:p0 + 64, :].rearrange("p (a b) -> p a b", a=8),
                                                   hmask[p0:p0 + 64, :].rearrange("p (a b) -> p a b", a=8), ALU.mult), [bpa, b_hmask], [b_AT])
                      for hd in range(8):
                          po, bpo = (PS[2], bPS[2]) if hd < 4 else (PS[3], bPS[3])
                          c0 = (hd % 4) * 128
                          t_(lambda h: h.matmul(po[p0:p0 + 64, c0:c0 + 128], AT[p0:p0 + 64, hd, 0:64], vtok[p0:p0 + 64, i, hd * 128:(hd + 1) * 128], start=True, stop=False),
                             [b_AT, b_vtok[i]], [bpo], inc=False)
                          t_(lambda h: h.matmul(po[p0:p0 + 64, c0:c0 + 128], qT[:, hd, cs], Sp[:, hd, :], start=False, stop=True),
                             [b_qT[hd], b_Sp], [bpo], inc=(hd % 4 == 3))
                      for hd in range(8):
                          pm, bpm = (PS[4], bPS[4]) if hd < 4 else (PS[5], bPS[5])
                          c0 = (hd % 4) * 128
                          t_(lambda h: h.matmul(pm[:, c0:c0 + 128], ktok[p0:p0 + 64, i, hd * 128:(hd + 1) * 128], vtok[p0:p0 + 64, i, hd * 128:(hd + 1) * 128], start=True, stop=True),
                             [b_ktok[i], b_vtok[i]], [bpm], inc=(hd % 4 == 3))
                      for hd in range(8):
                          pm, bpm = (PS[4], bPS[4]) if hd < 4 else (PS[5], bPS[5])
                          c0 = (hd % 4) * 128
                          v_(lambda h: h.tensor_scalar(Sst[:, hd, :], Sst[:, hd, :], csc[:, hd, ci, 1:2], None, ALU.mult), [b_csc], [b_S])
                          v_(lambda h: h.scalar_tensor_tensor(Sst[:, hd, :], pm[:, c0:c0 + 128], csc[:, hd, ci, 2:3], Sst[:, hd, :], ALU.mult, ALU.add),
                             [bpm, b_csc], [b_S])
                      if ci % 2 == 1:
                          gate_norm(t, i, [(PS[2], bPS[2]), (PS[3], bPS[3])])
                  out_proj(tiles)
              S.dma("sync", lambda h: h.dma_start(out=O["st_p"].rearrange("hh p v -> p hh v"), in_=Sst[:, :, :]), reads=[b_S], writes=[obuf], sembuf=b_S)
              S.barrier()

        TB = [(0, 512), (512, 512), (1024, 512), (1536, 512), (2048, 80)]

        def tcols(t):
            return slice(t * 128, t * 128 + rows(t))

        def norm_all(wname, hTf, b_hTf):
            for t in range(NT):
                rmsnorm_to_hT(t, wname, hTf[:, :, tcols(t)], b_hTf[t])

        def ffn(w_up, w_dn, F, row0_up, row0_dn, hTf, b_hTf, act, b_act, gate_ap, b_gate, ftmp, b_ftmp, gctr):
            nch = F // 128
            c0 = 0
            while c0 < nch:
                gs = min(4, nch - c0)
                gi = gctr[0] % 2
                gctr[0] += 1
                wa, bwa = wload(w_up[row0_up:row0_up + D, c0 * 128:(c0 + gs) * 128], 8, gs * 128)
                wb, bwb = wload(w_up[row0_up:row0_up + D, F + c0 * 128:F + (c0 + gs) * 128], 8, gs * 128)
                wd, bwd = wload(w_dn[row0_dn + c0 * 128:row0_dn + (c0 + gs) * 128, :], gs, D)
                for j in range(gs):
                    for bi, (cb, n) in enumerate(TB):
                        x = (j * len(TB) + bi) % 2
                        pa, bpa, pb, bpb = PS[2 * x], bPS[2 * x], PS[2 * x + 1], bPS[2 * x + 1]
                        rd = b_hTf[cb // 128:cb // 128 + (n + 127) // 128]
                        for k in range(8):
                            t_(lambda h: h.matmul(pa[:, :n], wa[:, k, j * 128:(j + 1) * 128], hTf[:, k, cb:cb + n], start=(k == 0), stop=(k == 7)),
                               [bwa] + rd, [bpa], inc=(k == 7))
                        for k in range(8):
                            t_(lambda h: h.matmul(pb[:, :n], wb[:, k, j * 128:(j + 1) * 128], hTf[:, k, cb:cb + n], start=(k == 0), stop=(k == 7)),
                               [bwb] + rd, [bpb], inc=(k == 7))
                        a_(lambda h: h.activation(ftmp[:, x, :n], pa[:, :n], AF.Silu), [bpa], [b_ftmp[x]])
                        v_(lambda h: h.tensor_tensor(act[:, gi, j, cb:cb + n], ftmp[:, x, :n], pb[:, :n], ALU.mult), [b_ftmp[x], bpb], [b_act[gi]])
                for t in range(NT):
                    r = rows(t)
                    for hf in range(2):
                        pz, bpz = PS[4 + hf], bPS[4 + hf]
                        for j in range(gs):
                            t_(lambda h: h.matmul(pz[:r, :], act[:, gi, j, tcols(t)], wd[:, j, hf * 512:(hf + 1) * 512], start=(j == 0), stop=(j == gs - 1)),
                               [b_act[gi], bwd], [bpz], inc=(j == gs - 1))
                        xs_ = X[:r, t, hf * 512:(hf + 1) * 512]
                        if gate_ap is None:
                            v_(lambda h: h.tensor_tensor(xs_, xs_, pz[:r, :], ALU.add), [bX[t], bpz], [bX[t]])
                        else:
                            v_(lambda h: h.scalar_tensor_tensor(xs_, pz[:r, :], gate_ap(t, r), xs_, ALU.mult, ALU.add), [bX[t], bpz, b_gate], [bX[t]])
                c0 += gs

        def ffn_phase(layer):
            with ExitStack() as ph:
                hTf = sb(ph, "hTf", [128, 8, NTOK], BF16); b_hTf = [Buf(f"hTf{t}") for t in range(NT)]
                act = sb(ph, "act", [128, 2, 4, NTOK], BF16); b_act = [Buf("act0"), Buf("act1")]
                ftmp = sb(ph, "ftmp", [128, 2, 512], F32); b_ftmp = [Buf("ftmp0"), Buf("ftmp1")]
                gctr = [0]
                load_wrow("ffn0" if layer == 0 else "ffn1", 0)
                norm_all("ffn0" if layer == 0 else "ffn1", hTf, b_hTf)
                if layer == 0:
                    ffn(I["ff_w_up"], I["ff_w_down"], DFF, 0, 0, hTf, b_hTf, act, b_act, None, None, ftmp, b_ftmp, gctr)
                else:
                    gates = sb(ph, "gates", [128, NT, 8], F32); b_gates = Buf("gates", True)
                    rt = sb(ph, "rt", [128, 8, 8], F32); b_rt_ = Buf("rt", True)
                    wr = sb(ph, "wr", [128, 8, 256], BF16); b_wr = Buf("wr")
                    v_(lambda h: h.memset(wr[:], 0.0), [], [b_wr])
                    brow = sb(ph, "brow", [128, 8], F32); b_brow = Buf("brow")
                    S.dma("gpsimd", lambda h: h.dma_start(out=wr[:, :, 0:8], in_=I["moe_w_router"].rearrange("(k p) n -> p k n", p=128)), writes=[b_wr])
                    S.dma("sync", lambda h: h.dma_start(out=brow[:], in_=I["moe_b_router"][0:1, :].partition_broadcast(128)), writes=[b_brow])
                    for t in range(NT):
                        r = rows(t)
                        pl, bpl = PS[5], bPS[5]
                        for k in range(8):
                            t_(lambda h: h.matmul(pl[:r, 0:256], hTf[:, k, tcols(t)], wr[:, k, :], start=(k == 0), stop=(k == 7)), [b_hTf[t], b_wr], [bpl], inc=(k == 7))
                        lg = rt[:r, 0, :]
                        v_(lambda h: h.tensor_tensor(lg, pl[:r, 0:8], brow[:r, :], ALU.add), [bpl, b_brow], [b_rt_])
                        v_(lambda h: h.max(rt[:r, 1, :], lg), [b_rt_], [b_rt_])
                        v_(lambda h: h.tensor_tensor(rt[:r, 2, 0:1], rt[:r, 1, 0:1], rt[:r, 1, 1:2], ALU.subtract), [b_rt_], [b_rt_])
                        a_(lambda h: h.activation(rt[:r, 2, 1:2], rt[:r, 2, 0:1], AF.Exp), [b_rt_], [b_rt_])
                        v_(lambda h: h.tensor_scalar(rt[:r, 2, 1:2], rt[:r, 2, 1:2], 1.0, None, ALU.add), [b_rt_], [b_rt_])
                        v_(lambda h: h.reciprocal(rt[:r, 2, 2:3], rt[:r, 2, 1:2]), [b_rt_], [b_rt_])
                        v_(lambda h: h.tensor_scalar(rt[:r, 2, 3:4], rt[:r, 2, 2:3], -1.0, 1.0, ALU.mult, ALU.add), [b_rt_], [b_rt_])
                        v_(lambda h: h.tensor_scalar(rt[:r, 3, :], lg, rt[:r, 1, 0:1], rt[:r, 2, 3:4], ALU.is_equal, ALU.mult), [b_rt_], [b_rt_])
                        v_(lambda h: h.tensor_scalar(rt[:r, 4, :], lg, rt[:r, 1, 1:2], rt[:r, 2, 2:3], ALU.is_equal, ALU.mult), [b_rt_], [b_rt_])
                        v_(lambda h: h.tensor_tensor(gates[:r, t, :], rt[:r, 3, :], rt[:r, 4, :], ALU.add), [b_rt_], [b_gates])
                    for e in range(8):
                        ffn(I["moe_w_up"], I["moe_w_down"], DFFE, e * D, e * DFFE, hTf, b_hTf, act, b_act,
                            (lambda t, r, e=e: gates[:r, t, e:e + 1]), b_gates, ftmp, b_ftmp, gctr)
                S.barrier()

        LAMBDA_INIT = 0.8 - 0.6 * float(np.exp(-0.3 * 1))
        NEGM = -30000.0
        wscr = nc.dram_tensor("wscr", [8, 384], F32, kind="Internal").ap()
        w2scr = nc.dram_tensor("w2scr", [384, 8], F32, kind="Internal").ap()
        b_wscr = Buf("wscr"); b_w2scr = Buf("w2scr")

        def dram_ap(base, offset, pat):
            return bass.AP(tensor=base.tensor, offset=offset, ap=pat)

        def sample_pass(hTf, qTt, b_qTt, kTt, b_kTt, b_small, NLAM, swb, b_swb, b_vp):
            S.force_same = True
            with ExitStack() as sp:
                KTall = hTf[:, :, :].rearrange("p a b -> p (a b)")[:, 0:16384].rearrange("p (a h k) -> p a h k", a=16, h=8)
                b_KT = [Buf(f"KT{p}") for p in range(16)]
                Vpg = lambda pg: ring[pg // 4][:, (pg % 4) * 1024:(pg % 4 + 1) * 1024]
                b_V = [bring[pg // 4] for pg in range(16)]
                Kraw = sb(sp, "Kraw", [128, 2, D], BF16); b_Kraw = [Buf("Kraw0"), Buf("Kraw1")]
                scs2 = [sb(sp, f"scs{i}", [8, 2052], F32) for i in range(2)]; b_scs2 = [Buf("scs0"), Buf("scs1")]
                Pbs2 = [sb(sp, f"Pbs{i}", [8, 2052], BF16) for i in range(2)]; b_Pbs2 = [Buf("Pbs0"), Buf("Pbs1")]
                PTs2 = [sb(sp, f"PTs{i}", [128, 16, 8], BF16) for i in range(2)]; b_PTs2 = [Buf("PTs0"), Buf("PTs1")]
                PTn2 = [sb(sp, f"PTn{i}", [4, 8], BF16) for i in range(2)]; b_PTn2 = [Buf("PTn0"), Buf("PTn1")]
                Qb2 = [sb(sp, f"Qb{i}", [128, 8], BF16) for i in range(2)]; b_Qb2 = [Buf("Qb0"), Buf("Qb1")]
                Bs = sb(sp, "Bs", [8, 8, 132], F32); b_Bs = Buf("Bs")
                vnew = sb(sp, "vnew", [4, D], BF16); b_vnew = Buf("vnew")
                oTs = sb(sp, "oTs", [128, 8, 64], BF16); b_oTs = Buf("oTs")
                o1s = sb(sp, "o1s", [4, 128], F32); b_o1s = Buf("o1s")
                ods = sb(sp, "ods", [4, 128], F32); b_ods = Buf("ods")
                osbs = sb(sp, "osbs", [4, 128], BF16); b_osbs = Buf("osbs")
                st32 = [sb(sp, f"st3{i}", [8, 16], F32) for i in range(2)]; b_st32 = [Buf("st30"), Buf("st31")]
                pti = sb(sp, "pti", [128, 256], I32); b_pti = Buf("pti")
                ptf = sb(sp, "ptf", [128, 256], F32); b_ptf = Buf("ptf")
                idx = sb(sp, "idx", [128, 256], I32); b_idx = Buf("idx")
                pio = sb(sp, "pio", [128, 1], F32); b_pio = Buf("pio")
                S.dma("sync", lambda h: h.dma_start(out=pio[:], in_=I["piota"][:, :]), writes=[b_pio])
                S.dma("sync", lambda h: h.dma_start(out=pti[:], in_=I["ptab"].rearrange("b (o p) -> o (b p)", o=1).partition_broadcast(128)), writes=[b_pti])
                v_(lambda h: h.tensor_copy(ptf[:], pti[:]), [b_pti], [b_ptf])
                v_(lambda h: h.tensor_scalar(ptf[:], ptf[:], 128.0, pio[:, 0:1], ALU.mult, ALU.add), [b_ptf, b_pio], [b_ptf])
                v_(lambda h: h.tensor_copy(idx[:], ptf[:]), [b_ptf], [b_idx])
                for m in range(2):
                    for q in range(4):
                        rw = m * 4 + q
                        S.dma("sync", lambda h: h.dma_start(out=Bs[rw:rw + 1, :, 0:128], in_=dram_ap(wscr, 127 - q, [[1, 1], [384, 8], [1, 128]])), reads=[b_wscr], writes=[b_Bs])
                        S.dma("sync", lambda h: h.dma_start(out=Bs[rw:rw + 1, :, 128:132], in_=dram_ap(wscr, 255 - q, [[1, 1], [384, 8], [1, 4]])), reads=[b_wscr], writes=[b_Bs])
                for b in range(16):
                    for pg in range(16):
                        j = pg % 2
                        c = b * 16 + pg
                        S.dma("gpsimd", lambda h: h.indirect_dma_start(out=Kraw[:, j, :], out_offset=None, in_=I["cache_k"][:, :],
                                                                       in_offset=bass.IndirectOffsetOnAxis(ap=idx[:, c:c + 1], axis=0)), reads=[b_idx], writes=[b_Kraw[j]])
                        S.dma("gpsimd", lambda h: h.indirect_dma_start(out=Vpg(pg), out_offset=None, in_=I["cache_v"][:, :],
                                                                       in_offset=bass.IndirectOffsetOnAxis(ap=idx[:, c:c + 1], axis=0)), reads=[b_idx], writes=[b_V[pg]])
                        for hd in range(8):
                            t_(lambda h: h.transpose(PSB[:, hd * 128:(hd + 1) * 128], Kraw[:, j, hd * 128:(hd + 1) * 128], ident_b[:, :]), [b_Kraw[j], b_idb], [bPSB], inc=(hd == 7))
                        a_(lambda h: h.copy(KTall[:, pg, :, :], PSB[:, :].rearrange("p (h k) -> p h k", h=8)), [bPSB], [b_KT[pg]])
                    S.dma("gpsimd", lambda h: h.dma_start(out=vnew[:, :], in_=O["v_s"][4 * b:4 * b + 4, :]), reads=[b_vp], writes=[b_vnew])
                    def samp_A(hd):
                            par = hd % 2
                            Qb, b_Qb, scs, b_scs, Pbs, b_Pbs, st3, b_st3 = Qb2[par], b_Qb2[par], scs2[par], b_scs2[par], Pbs2[par], b_Pbs2[par], st32[par], b_st32[par]
                            v_(lambda h: h.memset(Qb[:], 0.0), [], [b_Qb])
                            v_(lambda h: h.tensor_copy(Qb[0:64, 0:4], qTt[0:64, hd, 4 * b:4 * b + 4]), [b_qTt], [b_Qb])
                            v_(lambda h: h.tensor_copy(Qb[64:128, 4:8], qTt[64:128, hd, 4 * b:4 * b + 4]), [b_qTt], [b_Qb])
                            for cq in range(4):
                                pp_, bpp = PS[(cq + 2 * par) % 4], bPS[(cq + 2 * par) % 4]
                                t_(lambda h: h.matmul(pp_[0:8, :], Qb[:, :], KTall[:, 4 * cq:4 * cq + 4, hd, :], start=True, stop=True), [b_Qb] + b_KT[4 * cq:4 * cq + 4], [bpp])
                                a_(lambda h: h.activation(scs[:, cq * 512:(cq + 1) * 512], pp_[0:8, :], AF.Copy, scale=0.125), [bpp], [b_scs])
                            t_(lambda h: h.matmul(PS[4][0:8, 0:80], Qb[:, :], kTt[:, hd, 0:80], start=True, stop=True), [b_Qb, b_kTt], [bPS[4]])
                            a_(lambda h: h.activation(scs[:, 2048:2052], PS[4][0:8, 4 * b:4 * b + 4], AF.Copy, scale=0.125), [bPS[4]], [b_scs])
                            v_(lambda h: h.tensor_tensor(scs[:, 1920:2052], scs[:, 1920:2052], Bs[:, hd, :], ALU.add), [b_scs, b_Bs], [b_scs])
                            v_(lambda h: h.tensor_reduce(st3[:, 0:1], scs[:, :], AX.X, ALU.max), [b_scs], [b_st3])
                            v_(lambda h: h.tensor_scalar(st3[:, 1:2], st3[:, 0:1], -1.0, None, ALU.mult), [b_st3], [b_st3])
                            a_(lambda h: h.activation(Pbs[:, :], scs[:, :], AF.Exp, bias=st3[:, 1:2], scale=1.0, accum_out=st3[:, 2:3]), [b_scs, b_st3], [b_Pbs, b_st3])
                            v_(lambda h: h.reciprocal(st3[:, 3:4], st3[:, 2:3]), [b_st3], [b_st3])
                            v_(lambda h: h.tensor_scalar(Pbs[:, :], Pbs[:, :], st3[:, 3:4], None, ALU.mult), [b_Pbs, b_st3], [b_Pbs])

                    def samp_B(hd):
                            par = hd % 2
                            Pbs, b_Pbs, st3, b_st3, PTs, b_PTs, PTn, b_PTn = Pbs2[par], b_Pbs2[par], st32[par], b_st32[par], PTs2[par], b_PTs2[par], PTn2[par], b_PTn2[par]
                            c0 = 512 * par
                            for pg in range(16):
                                t_(lambda h: h.transpose(PSB[:, c0 + pg * 8:c0 + (pg + 1) * 8], Pbs[:, pg * 128:(pg + 1) * 128], ident_b[0:8, 0:8]), [b_Pbs, b_idb], [bPSB], inc=False)
                            t_(lambda h: h.transpose(PSB[0:4, c0 + 128:c0 + 136], Pbs[:, 2048:2052], ident_b[0:8, 0:8]), [b_Pbs, b_idb], [bPSB])
                            a_(lambda h: h.copy(PTs[:, :, :], PSB[:, c0:c0 + 128].rearrange("p (a b) -> p a b", a=16)), [bPSB], [b_PTs])
                            a_(lambda h: h.copy(PTn[:, :], PSB[0:4, c0 + 128:c0 + 136]), [bPSB], [b_PTn])
                            po, bpo = PS[5], bPS[5]
                            for m in range(2):
                                for pg in range(16):
                                    t_(lambda h: h.matmul(po[0:4, m * 128:(m + 1) * 128], PTs[:, pg, m * 4:(m + 1) * 4], Vpg(pg)[:, hd * 128:(hd + 1) * 128], start=(pg == 0), stop=False),
                                       [b_PTs, b_V[pg]], [bpo], inc=False)
                                t_(lambda h: h.matmul(po[0:4, m * 128:(m + 1) * 128], PTn[:, m * 4:(m + 1) * 4], vnew[:, hd * 128:(hd + 1) * 128], start=False, stop=True),
                                   [b_PTn, b_vnew], [bpo], inc=(m == 1))
                            a_(lambda h: h.copy(o1s[:, :], po[0:4, 0:128]), [bpo], [b_o1s])
                            v_(lambda h: h.scalar_tensor_tensor(ods[:, :], po[0:4, 128:256], NLAM[0:4, :], o1s[:, :], ALU.mult, ALU.add), [bpo, b_small, b_o1s], [b_ods])
                            a_(lambda h: h.activation(junk[0:4, 0:128], ods[:, :], AF.Square, accum_out=st3[0:4, 5:6]), [b_ods], [bjunk, b_st3])
                            a_(lambda h: h.activation(st3[0:4, 6:7], st3[0:4, 5:6], AF.Ln, scale=1.0 / 128, bias=epsc[0:4, 0:1]), [b_st3, b_epsc], [b_st3])
                            a_(lambda h: h.activation(st3[0:4, 7:8], st3[0:4, 6:7], AF.Exp, scale=-0.5), [b_st3], [b_st3])
                            v_(lambda h: h.scalar_tensor_tensor(osbs[:, :], ods[:, :], st3[0:4, 7:8], swb[0:4, :], ALU.mult, ALU.mult), [b_ods, b_st3, b_swb], [b_osbs])
                            t_(lambda h: h.transpose(PSB[:, c0 + 256:c0 + 260], osbs[:, :], ident_b[0:4, 0:4]), [b_osbs, b_idb], [bPSB])
                            a_(lambda h: h.copy(oTs[:, hd, 4 * b:4 * b + 4], PSB[:, c0 + 256:c0 + 260]), [bPSB], [b_oTs])

                    samp_A(0)
                    for hd_ in range(8):
                        if hd_ + 1 < 8:
                            samp_A(hd_ + 1)
                        samp_B(hd_)
                for hd in range(8):
                    wo, b_wo = wload(I["at_w_out"][hd * 128:(hd + 1) * 128, :], 1, D)
                    for hf in range(2):
                        t_(lambda h: h.matmul(PS[2 + hf][0:64, :], oTs[:, hd, :], wo[:, 0, hf * 512:(hf + 1) * 512], start=(hd == 0), stop=(hd == 7)),
                           [b_oTs, b_wo], [bPS[2 + hf]], inc=(hd == 7))
                for hf in range(2):
                    xs_ = X[0:64, TAIL, hf * 512:(hf + 1) * 512]
                    v_(lambda h: h.tensor_tensor(xs_, xs_, PS[2 + hf][0:64, :], ALU.add), [bX[TAIL], bPS[2 + hf]], [bX[TAIL]])
                S.barrier()
            S.force_same = False

        def attn_phase():
            with ExitStack() as ph:
                load_wrow("mix1", 0)
                hTf = sb(ph, "hTf", [128, 8, NTOK], BF16); b_hTf = [Buf(f"ahTf{t}") for t in range(NT)]
                norm_all("mix1", hTf, b_hTf)
                if att_stop == 3:
                    S.barrier()
                    return
                qTt = sb(ph, "qTt", [128, 8, 80], BF16); b_qTt = Buf("qTt")
                kTt = sb(ph, "kTt", [128, 8, 80], BF16); b_kTt = Buf("kTt")
                small = sb(ph, "small", [128, 64], F32); b_small = Buf("small", True)
                swb = sb(ph, "swb", [128, 128], F32); b_swb = Buf("swb", True)
                ones_f = sb(ph, "ones_f", [128, 128], F32); b_ones = Buf("ones_f")
                ones_b = sb(ph, "ones_b", [128, 8], BF16); b_onesb = Buf("ones_b")
                v_(lambda h: h.memset(ones_f[:], 1.0), [], [b_ones])

                v_(lambda h: h.memset(ones_b[:], 1.0), [], [b_onesb])
                jflip = sb(ph, "jflipb", [128, 128], BF16); b_jflip = Buf("jflipb")
                su = ExitStack()
                lv = sb(su, "lv", [128, 4, 64], F32); b_lv = Buf("lv", True)
                for i, nm in enumerate(("at_lambda_q1", "at_lambda_k1", "at_lambda_q2", "at_lambda_k2")):
                    S.dma("sync", lambda h: h.dma_start(out=lv[:, i, :], in_=I[nm][0:1, :].partition_broadcast(128)), writes=[b_lv])
                v_(lambda h: h.tensor_tensor(lv[:, 0, :], lv[:, 0, :], lv[:, 1, :], ALU.mult), [b_lv], [b_lv])
                v_(lambda h: h.tensor_tensor(lv[:, 2, :], lv[:, 2, :], lv[:, 3, :], ALU.mult), [b_lv], [b_lv])
                v_(lambda h: h.tensor_reduce(small[:, 0:1], lv[:, 0, :], AX.X, ALU.add), [b_lv], [b_small])
                v_(lambda h: h.tensor_reduce(small[:, 1:2], lv[:, 2, :], AX.X, ALU.add), [b_lv], [b_small])
                a_(lambda h: h.activation(small[:, 2:4], small[:, 0:2], AF.Exp), [b_small], [b_small])
                v_(lambda h: h.tensor_tensor(small[:, 4:5], small[:, 3:4], small[:, 2:3], ALU.subtract), [b_small], [b_small])
                v_(lambda h: h.tensor_scalar(small[:, 5:6], small[:, 4:5], -LAMBDA_INIT, None, ALU.add), [b_small], [b_small])
                NLAM = small[:, 5:6]
                S.dma("sync", lambda h: h.dma_start(out=swb[:], in_=I["at_subln_w"][0:1, :].partition_broadcast(128)), writes=[b_swb])
                v_(lambda h: h.tensor_scalar(swb[:], swb[:], 1.0 - LAMBDA_INIT, None, ALU.mult), [b_swb], [b_swb])
                if att_stop == 4:
                    S.barrier()
                    return
                tabx = sb(su, "tabx", [64, 8], F32); b_tabx = Buf("tabx", True)
                tabh = sb(su, "tabh", [64, 2, 8], BF16); b_tabh = Buf("tabh", True)
                tabf = sb(su, "tabf", [64, 8], F32); b_tabf = Buf("tabf", True)
                oh = sb(su, "oh", [64, 384], F32); b_oh = Buf("oh", True)
                ohb = sb(su, "ohb", [64, 384], BF16); b_ohb = Buf("ohb", True)
                v_(lambda h: h.memset(tabx[:, :], NEGM), [], [b_tabx])
                S.dma("sync", lambda h: h.dma_start(out=tabx[0:32, :], in_=I["rel_bias_table"][:, :]), writes=[b_tabx])
                S.dma("sync", lambda h: h.dma_start(out=oh[0:33, :], in_=I["oh"][:, :]), writes=[b_oh])
                S.dma("gpsimd", lambda h: h.dma_start(out=jflip[:], in_=I["jflip"][:, :]), writes=[b_jflip])
                v_(lambda h: h.tensor_copy(ohb[0:33, :], oh[0:33, :]), [b_oh], [b_ohb])
                v_(lambda h: h.tensor_copy(tabh[:, 0, :], tabx[:, :]), [b_tabx], [b_tabh])
                v_(lambda h: h.tensor_copy(tabf[:, :], tabh[:, 0, :]), [b_tabh], [b_tabf])
                v_(lambda h: h.tensor_tensor(tabh[:, 1, :], tabx[:, :], tabf[:, :], ALU.subtract), [b_tabx, b_tabf], [b_tabh])
                wsb = sb(su, "wsb", [128, 3, 8], F32); b_wsb = Buf("wsb", True)
                t_(lambda h: h.matmul(PS[5][0:8, 0:384], tabh[0:33, 0, :], ohb[0:33, :], start=True, stop=False), [b_tabh, b_ohb], [bPS[5]], inc=False)
                t_(lambda h: h.matmul(PS[5][0:8, 0:384], tabh[0:33, 1, :], ohb[0:33, :], start=False, stop=True), [b_tabh, b_ohb], [bPS[5]])
                a_(lambda h: h.copy(junk[0:8, 0:384], PS[5][0:8, 0:384]), [bPS[5]], [bjunk])
                S.dma("sync", lambda h: h.dma_start(out=wscr[:, :], in_=junk[0:8, 0:384]), reads=[bjunk], writes=[b_wscr])
                for i3 in range(3):
                    t_(lambda h: h.matmul(PS[5][:, 400 + i3 * 8:408 + i3 * 8], ohb[0:33, i3 * 128:(i3 + 1) * 128], tabh[0:33, 0, :], start=True, stop=False),
                       [b_tabh, b_ohb], [bPS[5]], inc=False)
                    t_(lambda h: h.matmul(PS[5][:, 400 + i3 * 8:408 + i3 * 8], ohb[0:33, i3 * 128:(i3 + 1) * 128], tabh[0:33, 1, :], start=False, stop=True),
                       [b_tabh, b_ohb], [bPS[5]])
                a_(lambda h: h.copy(wsb[:, :, :], PS[5][:, 400:424].rearrange("p (a b) -> p a b", a=3)), [bPS[5]], [b_wsb])
                S.dma("sync", lambda h: h.dma_start(out=w2scr.rearrange("(a p) n -> p a n", p=128), in_=wsb[:, :, :]), reads=[b_wsb], writes=[b_w2scr])

                S.barrier()
                su.close()
                if att_stop == 1:
                    S.barrier()
                    return
                b_vp = Buf("dram_vp")
                with ExitStack() as kvp:
                    kvo = sb(kvp, "kvo", [128, 2, 512], F32); b_kvo = [Buf("kvo0"), Buf("kvo1")]
                    ctr = 0
                    for cbk in range(4):
                        wkk, bwkk = wload(I["at_w_qkv"][:, D + cbk * 512:D + (cbk + 1) * 512], 8, 512)
                        isv = cbk >= 2
                        ocs = slice((cbk % 2) * 512, (cbk % 2 + 1) * 512)
                        for t in range(NT):
                            r = rows(t)
                            j = ctr % 2
                            ctr += 1
                            pk, bpk = PS[2 + j], bPS[2 + j]
                            for k in range(8):
                                t_(lambda h: h.matmul(pk[:r, :], hTf[:, k, tcols(t)], wkk[:, k, :], start=(k == 0), stop=(k == 7)), [bwkk, b_hTf[t]], [bpk], inc=(k == 7))
                            a_(lambda h: h.copy(kvo[:r, j, :], pk[:r, :]), [bpk], [b_kvo[j]])
                            dp = O["v_p"] if isv else O["k_p"]
                            ds_ = O["v_s"] if isv else O["k_s"]
                            wr_ = [b_vp] if isv else [obuf]
                            if t < 16:
                                S.dma("sync", lambda h: h.dma_start(out=dp[16 + t * 128:16 + (t + 1) * 128, ocs], in_=kvo[:, j, :]), reads=[b_kvo[j]], writes=wr_, sembuf=b_kvo[j])
                            else:
                                S.dma("sync", lambda h: h.dma_start(out=ds_[:, ocs], in_=kvo[0:64, j, :]), reads=[b_kvo[j]], writes=wr_, sembuf=b_kvo[j])
                                S.dma("sync", lambda h: h.dma_start(out=dp[0:16, ocs], in_=kvo[64:80, j, :]), reads=[b_kvo[j]], writes=wr_, sembuf=b_kvo[j])
                    S.barrier()
                with ExitStack() as pp:
                    qTh = sb(pp, "qTh", [128, NTOK], BF16); b_qTh = Buf("qTh")
                    kTh = sb(pp, "kTh", [128, NTOK], BF16); b_kTh = Buf("kTh")
                    vh = sb(pp, "vh", [128, NT + 1, 128], BF16); b_vh = Buf("vh")
                    vhf = vh[:, :, :].rearrange("p a b -> p (a b)")
                    vmeta = sb(pp, "vmeta", [16, 256], BF16); b_vmeta = Buf("vmeta")
                    v_(lambda h: h.memset(vh[:], 0.0), [], [b_vh])
                    v_(lambda h: h.memset(vmeta[:], 0.0), [], [b_vmeta])
                    sc2 = [sb(pp, f"sc{m}", [128, 2064], F32) for m in range(2)]; b_sc2 = [Buf("sc0"), Buf("sc1")]
                    Pb2 = [sb(pp, f"Pb{m}", [128, 2064], BF16) for m in range(2)]; b_Pb2 = [Buf("Pb0"), Buf("Pb1")]
                    PT2 = [sb(pp, f"PT{m}", [128, 16, 128], BF16) for m in range(2)]; b_PT2 = [Buf("PT0"), Buf("PT1")]
                    PTm2 = [sb(pp, f"PTm{m}", [16, 128], BF16) for m in range(2)]; b_PTm2 = [Buf("PTm0", True), Buf("PTm1", True)]
                    st22 = [sb(pp, f"st2{m}", [128, 16], F32) for m in range(2)]; b_st22 = [Buf("st20", True), Buf("st21", True)]
                    sc, b_sc = sc2[1], b_sc2[1]
                    oTh = sb(pp, "oTh", [128, 2048], BF16); b_oTh = Buf("oTh")
                    Bh = sb(pp, "Bh", [128, 256], F32); b_Bh = Buf("Bh", True)
                    Bf = sb(pp, "Bf", [128, 272], F32); b_Bf = Buf("Bf", True)
                    Bfh = sb(pp, "Bfh", [128, 2, 272], BF16); b_Bfh = Buf("Bfh", True)
                    Bm0 = sb(pp, "Bm0", [128, 16], F32); b_Bm0 = Buf("Bm0", True)
                    o1 = sb(pp, "o1", [128, 128], F32); b_o1 = Buf("o1", True)
                    od = sb(pp, "od", [128, 128], F32); b_od = Buf("od", True)
                    osb = sb(pp, "osb", [128, 128], BF16); b_osb = Buf("osb", True)
                    st2 = sb(pp, "st2", [128, 16], F32); b_st2 = Buf("st2", True)
                    Wq = I["at_w_qkv"]
                    for hd in range(8):
                        wq, bwq = wload(Wq[:, hd * 128:(hd + 1) * 128], 8, 128)
                        wk, bwk = wload(Wq[:, D + hd * 128:D + (hd + 1) * 128], 8, 128)
                        S.dma("sync", lambda h: h.dma_start(out=Bf[:, 0:256], in_=dram_ap(wscr, hd * 384, [[1, 128], [1, 256]])), reads=[b_wscr], writes=[b_Bf])
                        S.dma("sync", lambda h: h.dma_start(out=Bf[:, 256:272], in_=dram_ap(wscr, hd * 384 + 112, [[1, 128], [1, 16]])), reads=[b_wscr], writes=[b_Bf])
                        v_(lambda h: h.tensor_copy(Bfh[:, 0, :], Bf[:, :]), [b_Bf], [b_Bfh])
                        v_(lambda h: h.tensor_tensor(Bfh[:, 1, :], Bf[:, :], Bfh[:, 0, :], ALU.subtract), [b_Bf, b_Bfh], [b_Bfh])
                        t_(lambda h: h.matmul(PS[5][:, 0:272], jflip[:, :], Bfh[:, 0, :], start=True, stop=False), [b_jflip, b_Bfh], [bPS[5]], inc=False)
                        t_(lambda h: h.matmul(PS[5][:, 0:272], jflip[:, :], Bfh[:, 1, :], start=False, stop=True), [b_jflip, b_Bfh], [bPS[5]])
                        a_(lambda h: h.copy(Bh[:, :], PS[5][:, 0:256]), [bPS[5]], [b_Bh])
                        a_(lambda h: h.copy(Bm0[:, :], PS[5][:, 256:272]), [bPS[5]], [b_Bm0])
                        if att_stop == 5:
                            break
                        TBA = [(0, 512), (512, 512), (1024, 512), (1536, 512), (1872, 256)]
                        for bi, (cb, n) in enumerate(TBA):
                            rd = b_hTf[cb // 128:(cb + n + 127) // 128]
                            for (wsrc, bws, pi) in ((wq, bwq, 0), (wk, bwk, 1)):
                                pp_, bpp = PS[pi], bPS[pi]
                                for k in range(8):
                                    t_(lambda h: h.matmul(pp_[:, :n], wsrc[:, k, :], hTf[:, k, cb:cb + n], start=(k == 0), stop=(k == 7)), [bws] + rd, [bpp], inc=(k == 7))
                                if pi == 0:
                                    if bi < 4:
                                        a_(lambda h: h.copy(qTh[:, cb:cb + n], pp_[:, :n]), [bpp], [b_qTh])
                                    else:
                                        v_(lambda h: h.tensor_copy(qTt[:, hd, :], pp_[:, 176:256]), [bpp], [b_qTt])
                                else:
                                    if bi < 4:
                                        a_(lambda h: h.copy(kTh[:, 16 + cb:16 + cb + n], pp_[:, :n]), [bpp], [b_kTh])
                                    else:
                                        a_(lambda h: h.copy(kTh[:, 0:16], pp_[:, 240:256]), [bpp], [b_kTh])
                                        v_(lambda h: h.tensor_copy(kTt[:, hd, :], pp_[:, 176:256]), [bpp], [b_kTt])
                        if att_stop == 6:
                            break
                        cs_ = slice(hd * 128, (hd + 1) * 128)
                        S.dma("gpsimd", lambda h: h.dma_start(out=vh[:, 0:16, :], in_=O["v_p"][16:16 + 2048, cs_].rearrange("(t p) n -> p t n", p=128)), reads=[b_vp], writes=[b_vh])
                        S.dma("gpsimd", lambda h: h.dma_start(out=vmeta[:, 0:128], in_=O["v_p"][0:16, cs_]), reads=[b_vp], writes=[b_vmeta])
                        if att_stop == 2:
                            break
                        MC = 2048 + 64
                        def unit_A(qt, m):
                                L0 = (qt + 1) * 128
                                L = 16 + L0
                                nb_ = (L + 511) // 512
                                bsz = (L + nb_ - 1) // nb_
                                blocks = [(c, min(bsz, L - c)) for c in range(0, L, bsz)]
                                ms = slice(m * 64, (m + 1) * 64)
                                scm, b_scm, Pbm, b_Pbm, stm, b_stm = sc2[m], b_sc2[m], Pb2[m], b_Pb2[m], st22[m], b_st22[m]
                                for bi, (kc, n) in enumerate(blocks):
                                    pp_, bpp = PS[(bi + 2 * m) % 4], bPS[(bi + 2 * m) % 4]
                                    t_(lambda h: h.matmul(pp_[:, :n], qTh[ms, qt * 128:(qt + 1) * 128], kTh[ms, kc:kc + n], start=True, stop=True), [b_qTh, b_kTh], [bpp])
                                    a_(lambda h: h.activation(scm[:, kc:kc + n], pp_[:, :n], AF.Copy, scale=0.125), [bpp], [b_scm])
                                if qt == 0:
                                    v_(lambda h: h.tensor_tensor(scm[:, 0:16], scm[:, 0:16], Bm0[:, :], ALU.add), [b_scm, b_Bm0], [b_scm])
                                    v_(lambda h: h.tensor_tensor(scm[:, 16:144], scm[:, 16:144], Bh[:, 128:256], ALU.add), [b_scm, b_Bh], [b_scm])
                                else:
                                    v_(lambda h: h.tensor_tensor(scm[:, L - 256:L], scm[:, L - 256:L], Bh[:, :], ALU.add), [b_scm, b_Bh], [b_scm])
                                v_(lambda h: h.tensor_reduce(stm[:, 0:1], scm[:, :L], AX.X, ALU.max), [b_scm], [b_stm])
                                v_(lambda h: h.tensor_scalar(stm[:, 1:2], stm[:, 0:1], -1.0, None, ALU.mult), [b_stm], [b_stm])
                                a_(lambda h: h.activation(Pbm[:, :L], scm[:, :L], AF.Exp, bias=stm[:, 1:2], scale=1.0, accum_out=stm[:, 2:3]), [b_scm, b_stm], [b_Pbm, b_stm])
                                v_(lambda h: h.reciprocal(stm[:, 3:4], stm[:, 2:3]), [b_stm], [b_stm])

                        def unit_B(qt, m):
                                L0 = (qt + 1) * 128
                                L = 16 + L0
                                nb_ = (L + 511) // 512
                                bsz = (L + nb_ - 1) // nb_
                                blocks = [(c, min(bsz, L - c)) for c in range(0, L, bsz)]
                                Pbm, b_Pbm, stm, b_stm, PTq, b_PTq, PTmm, b_PTmm = Pb2[m], b_Pb2[m], st22[m], b_st22[m], PT2[m], b_PT2[m], PTm2[m], b_PTm2[m]
                                t_(lambda h: h.transpose(PSB[:16, 896:1024], Pbm[:, 0:16], ident_b[:, :]), [b_Pbm, b_idb], [bPSB])
                                a_(lambda h: h.copy(PTmm[:, :], PSB[:16, 896:1024]), [bPSB], [b_PTmm])
                                for j0 in range(0, qt + 1, 7):
                                    nj = min(7, qt + 1 - j0)
                                    for jj in range(nj):
                                        t_(lambda h: h.transpose(PSB[:, jj * 128:(jj + 1) * 128], Pbm[:, 16 + (j0 + jj) * 128:16 + (j0 + jj + 1) * 128], ident_b[:, :]),
                                           [b_Pbm, b_idb], [bPSB], inc=(jj == nj - 1))
                                    if (j0 // 7) % 2 == 0:
                                        a_(lambda h: h.copy(PTq[:, j0:j0 + nj, :], PSB[:, 0:nj * 128].rearrange("p (a b) -> p a b", a=nj)), [bPSB], [b_PTq])
                                    else:
                                        v_(lambda h: h.tensor_copy(PTq[:, j0:j0 + nj, :], PSB[:, 0:nj * 128].rearrange("p (a b) -> p a b", a=nj)), [bPSB], [b_PTq])
                                po, bpo = PS[4 + m], bPS[4 + m]
                                t_(lambda h: h.matmul(po[:, 0:256], PTmm[:, :], vmeta[:, :], start=True, stop=False), [b_PTmm, b_vmeta], [bpo], inc=False)
                                for j in range(qt + 1):
                                    t_(lambda h: h.matmul(po[:, 0:256], PTq[:, j, :], vhf[:, j * 128:j * 128 + 256], start=False, stop=(j == qt)), [b_PTq, b_vh], [bpo], inc=(j == qt))
                                if m == 0:
                                    v_(lambda h: h.tensor_scalar(o1[:, :], po[:, 0:128], stm[:, 3:4], None, ALU.mult), [bpo, b_stm], [b_o1])
                                else:
                                    v_(lambda h: h.tensor_tensor(stm[:, 4:5], stm[:, 3:4], NLAM, ALU.mult), [b_stm, b_small], [b_stm])
                                    v_(lambda h: h.scalar_tensor_tensor(od[:, :], po[:, 0:128], stm[:, 4:5], o1[:, :], ALU.mult, ALU.add), [bpo, b_stm, b_o1], [b_od])

                        def unit_C(qt):
                                a_(lambda h: h.activation(junk[:, 0:128], od[:, :], AF.Square, accum_out=st2[:, 5:6]), [b_od], [bjunk, b_st2])
                                a_(lambda h: h.activation(st2[:, 6:7], st2[:, 5:6], AF.Ln, scale=1.0 / 128, bias=epsc[:, 0:1]), [b_st2, b_epsc], [b_st2])
                                a_(lambda h: h.activation(st2[:, 7:8], st2[:, 6:7], AF.Exp, scale=-0.5), [b_st2], [b_st2])
                                v_(lambda h: h.scalar_tensor_tensor(osb[:, :], od[:, :], st2[:, 7:8], swb[:, :], ALU.mult, ALU.mult), [b_od, b_st2, b_swb], [b_osb])
                                t_(lambda h: h.transpose(PSB[:, 0:128], osb[:, :], ident_b[:, :]), [b_osb, b_idb], [bPSB])
                                a_(lambda h: h.copy(oTh[:, qt * 128:(qt + 1) * 128], PSB[:, 0:128]), [bPSB], [b_oTh])

                        units = [(qt, m) for qt in range(16) for m in range(2)]
                        unit_A(*units[0])
                        for ui, (qt, m) in enumerate(units):
                            if ui + 1 < len(units):
                                unit_A(*units[ui + 1])
                            unit_B(qt, m)
                            if m == 1:
                                unit_C(qt)
                        if hd == 7:
                            dump("oTh7", oTh[:, :], [128, 2048], [b_oTh], BF16)
                            dump("sc7", sc[:, :], [128, 2064], [b_sc])
                            dump("Bh7", Bh[:, :], [128, 256], [b_Bh])
                            dump("Bm07", Bm0[:, :], [128, 16], [b_Bm0])
                            dump("st27", st2[:, :], [128, 16], [b_st2])
                            dump("small", small[:, :], [128, 64], [b_small])
                        wo, b_wo = wload(I["at_w_out"][hd * 128:(hd + 1) * 128, :], 1, D)
                        for t in range(16):
                            for hf in range(2):
                                pz, bpz = PS[5], bPS[5]
                                t_(lambda h: h.matmul(pz[:, :], oTh[:, t * 128:(t + 1) * 128], wo[:, 0, hf * 512:(hf + 1) * 512], start=True, stop=True), [b_oTh, b_wo], [bpz])
                                xs_ = X[:, t, hf * 512:(hf + 1) * 512]
                                v_(lambda h: h.tensor_tensor(xs_, xs_, pz[:, :], ALU.add), [bX[t], bpz], [bX[t]])
                    S.barrier()
                if do_sample:
                    sample_pass(hTf, qTt, b_qTt, kTt, b_kTt, b_small, NLAM, swb, b_swb, b_vp)
                S.barrier()

        if stage >= 2 and not skip0:
            ffn_phase(0)

        if stage >= 3:
            attn_phase()
        if stage >= 4:
            ffn_phase(1)
        if stage >= 5:
            load_wrow("fin", 0)
            with ExitStack() as ph:
                yo = sb(ph, "yo", [128, 2, D], F32); b_yo = [Buf("yo0"), Buf("yo1")]
                for t in range(NT):
                    r = 64 if t == TAIL else 128
                    j = t % 2
                    ss = stat[:r, 0:1]
                    a_(lambda h: h.activation(junk[:r, :], X[:r, t, :], AF.Square, accum_out=ss), [bX[t]], [bjunk, bstat])
                    a_(lambda h: h.activation(stat[:r, 2:3], ss, AF.Ln, scale=1.0 / D, bias=epsc[:r, 0:1]), [bstat, b_epsc], [bstat])
                    a_(lambda h: h.activation(stat[:r, 3:4], stat[:r, 2:3], AF.Exp, scale=-0.5), [bstat], [bstat])
                    v_(lambda h: h.scalar_tensor_tensor(yo[:r, j, :], X[:r, t, :], stat[:r, 3:4], wrow[:r, WR["fin"], :], ALU.mult, ALU.mult),
                       [bX[t], bstat, b_wrow[WR["fin"]]], [b_yo[j]])
                    dst = O["y_s"][:, :] if t == TAIL else O["y_p"][t * 128:(t + 1) * 128, :]
                    S.dma("sync", lambda h: h.dma_start(out=dst, in_=yo[:r, j, :]), reads=[b_yo[j]], writes=[obuf], sembuf=b_yo[j])
                S.barrier()

        if dbg:
            for t in range(NT):
                S.dma("sync", lambda h: h.dma_start(out=O["dbg_x"][t * 128:(t + 1) * 128, :], in_=X[:, t, :]), reads=[bX[t]], writes=[obuf], sembuf=bX[t])
        S.barrier()
        print("sched stats", {n: (e.n_inst, e.n_wait) for n, e in S.E.items()}, "nsem", S.nsem)
    return nc


def make_in_maps(inputs):
    consts = host_consts()
    f = lambda a: np.ascontiguousarray(a, dtype=np.float32)
    shared = {
        "cache_k": f(inputs["cache_k"]).reshape(N_PHYS * 128, D),
        "cache_v": f(inputs["cache_v"]).reshape(N_PHYS * 128, D),
        "meta_tokens": f(inputs["meta_tokens"]),
        "norm_mix_w": f(inputs["norm_mix_w"]), "norm_ffn_w": f(inputs["norm_ffn_w"]),
        "hg_w_in": f(inputs["hg_w_in"][0]), "hg_lower_bound": f(inputs["hg_lower_bound"]),
        "hg_norm_w": f(inputs["hg_norm_w"]), "hg_w_out": f(inputs["hg_w_out"][0]),
        "at_w_qkv": f(inputs["at_w_qkv"][0]),
        "at_lambda_q1": f(inputs["at_lambda_q1"]), "at_lambda_k1": f(inputs["at_lambda_k1"]),
        "at_lambda_q2": f(inputs["at_lambda_q2"]), "at_lambda_k2": f(inputs["at_lambda_k2"]),
        "at_subln_w": f(inputs["at_subln_w"]), "at_w_out": f(inputs["at_w_out"][0]),
        "rel_bias_table": f(inputs["rel_bias_table"]),
        "ff_w_up": f(inputs["ff_w_up"][0]), "ff_w_down": f(inputs["ff_w_down"][0]),
        "moe_w_router": f(inputs["moe_w_router"][0]), "moe_b_router": f(inputs["moe_b_router"]),
        "moe_w_up": f(inputs["moe_w_up"][0]).reshape(8 * D, 2 * DFFE),
        "moe_w_down": f(inputs["moe_w_down"][0]).reshape(8 * DFFE, D),
        "final_norm_w": f(inputs["final_norm_w"]).reshape(1, D),
    }
    shared.update(consts)
    maps = []
    for c in range(NCORES):
        m = dict(shared)
        m["xp"] = f(inputs["x_prompt"][c])
        m["xs"] = f(inputs["x_sample"][16 * c:16 * c + 16]).reshape(64, D)
        m["state"] = f(inputs["state_hgrn"][0, 16 * c:16 * c + 16])
        m["ptab"] = np.ascontiguousarray(inputs["page_table"][16 * c:16 * c + 16], dtype=np.int32)
        maps.append(m)
    return maps


def kernel(**inputs):
    nc = build()
    maps = make_in_maps(inputs)
    res = run_bass_kernel_spmd(nc, maps, core_ids=list(range(NCORES)))
    R = res.results
    y_p = np.stack([R[c]["y_p"] for c in range(NCORES)])
    y_s = np.concatenate([R[c]["y_s"].reshape(16, 4, D) for c in range(NCORES)])
    st_p = np.stack([R[c]["st_p"] for c in range(NCORES)])[None]
    st_s = np.concatenate([R[c]["st_s"] for c in range(NCORES)])[None]
    k_p = np.stack([R[c]["k_p"].reshape(2064, 8, 2, 64) for c in range(NCORES)])[None]
    v_p = np.stack([R[c]["v_p"].reshape(2064, 8, 128) for c in range(NCORES)])[None]
    k_s = np.concatenate([R[c]["k_s"].reshape(16, 4, 8, 2, 64) for c in range(NCORES)])[None]
    v_s = np.concatenate([R[c]["v_s"].reshape(16, 4, 8, 128) for c in range(NCORES)])[None]
    return (y_p, y_s, st_p, st_s, k_p, v_p, k_s, v_s)
```

```python
import os
from contextlib import ExitStack
import numpy as np
import ml_dtypes
import concourse.bass as bass
import concourse.mybir as mybir
from concourse.bass_utils import run_bass_kernel_spmd

F32 = mybir.dt.float32
BF16 = mybir.dt.bfloat16
I32 = mybir.dt.int32
ALU = mybir.AluOpType
AF = mybir.ActivationFunctionType
AX = mybir.AxisListType

NCORES = 8
D = 1024
NT = 17
TAIL = 16
NTOK = 2048 + 80
EPS = 1e-6
N_PHYS = 2560
DFF = 2816
DFFE = 3584
SEM_LIMIT = 30000


class Buf:
    __slots__ = ("name", "w", "r", "dsem", "dcnt", "strict")

    def __init__(self, name, strict=False):
        self.name = name
        self.strict = strict
        self.w = None
        self.r = []
        self.dsem = None
        self.dcnt = 0


class Eng:
    def __init__(self, name, h):
        self.name = name
        self.h = h
        self.sem = None
        self.cnt = 0
        self.seen = {}
        self.pend_r = []
        self.pend_w = []
        self.n_inst = 0
        self.n_wait = 0


class Sched:
    def __init__(self, nc, stack):
        self.nc = nc
        self.stack = stack
        self.nsem = 0
        self.E = {}
        self.dbufs = []
        self.old_sems = []
        self.force_same = False
        for name in ("tensor", "vector", "scalar", "gpsimd", "sync"):
            e = Eng(name, getattr(nc, name))
            self.E[name] = e
            self._new_sem(e)

    def _alloc_sem(self, name):
        self.nsem += 1
        return self.stack.enter_context(self.nc.semaphore(f"{name}_{self.nsem}"))

    def _new_sem(self, e):
        if e.sem is not None:
            self.old_sems.append((e.sem, e.cnt, e.name))
        e.sem = self._alloc_sem("e_" + e.name)
        e.cnt = 0

    def _wait(self, e, tok, strict=False):
        sem, val, owner = tok
        if owner == e.name and not (strict or self.force_same):
            return
        k = id(sem)
        if e.seen.get(k, 0) >= val:
            return
        e.h.wait_ge(sem, val)
        e.seen[k] = val
        e.n_wait += 1

    def _deps(self, e, reads, writes):
        for b in reads:
            if b.w is not None:
                self._wait(e, b.w, b.strict)
        for b in writes:
            if b.w is not None:
                self._wait(e, b.w, b.strict)
            for t in b.r:
                self._wait(e, t)

    @staticmethod
    def _compact(toks):
        best = {}
        for t in toks:
            k = id(t[0])
            if k not in best or best[k][1] < t[1]:
                best[k] = t
        return list(best.values())

    def op(self, eng, fn, reads=(), writes=(), inc=True):
        e = self.E[eng]
        self._deps(e, reads, writes)
        inst = fn(e.h)
        e.n_inst += 1
        e.pend_r.extend(reads)
        e.pend_w.extend(writes)
        if inc:
            if e.cnt >= SEM_LIMIT:
                self._new_sem(e)
            e.cnt += 1
            inst.then_inc(e.sem, 1)
            tok = (e.sem, e.cnt, e.name)
            for b in e.pend_w:
                b.w = tok
                b.r = []
            for b in e.pend_r:
                if b.w is not tok:
                    b.r.append(tok)
                    if len(b.r) > 10:
                        b.r = self._compact(b.r)
            e.pend_r = []
            e.pend_w = []
        return inst

    def dma(self, q, fn, reads=(), writes=(), sembuf=None):
        e = self.E[q]
        self._deps(e, reads, writes)
        sb = sembuf or (writes[0] if writes else reads[0])
        if sb.dsem is None or sb.dcnt + 16 > 60000:
            sb.dsem = self._alloc_sem("d_" + sb.name)
            sb.dcnt = 0
            if sb not in self.dbufs:
                self.dbufs.append(sb)
        inst = fn(e.h)
        e.n_inst += 1
        sb.dcnt += 16
        inst.then_inc(sb.dsem, 16)
        tok = (sb.dsem, sb.dcnt, None)
        for b in writes:
            b.w = tok
            b.r = []
        for b in reads:
            b.r.append(tok)
            if len(b.r) > 10:
                b.r = self._compact(b.r)
        return inst

    def wait_all(self, eng, bufs):
        e = self.E[eng]
        for b in bufs:
            if b.w is not None:
                self._wait(e, b.w)
            for t in b.r:
                self._wait(e, t)

    def barrier(self):
        toks = [(e.sem, e.cnt, e.name) for e in self.E.values() if e.cnt > 0]
        toks += [(b.dsem, b.dcnt, None) for b in self.dbufs if b.dcnt > 0]
        for e in self.E.values():
            for t in toks:
                self._wait(e, t)


def _bucket(n):
    n = max(int(n), 0)
    if n < 16:
        return n
    nf = np.float32(max(n, 1))
    large = 16 + int(np.float32(np.float32(np.log(np.float32(nf / np.float32(16)))) / np.float32(np.log(8.0))) * np.float32(16))
    return min(large, 31)


def host_consts():
    c = {}
    c["ident_f"] = np.eye(128, dtype=np.float32)
    m = (np.arange(64)[:, None] <= np.arange(64)[None, :]).astype(np.float32)
    c["hmask"] = np.tile(np.tile(m, (1, 8)), (2, 1))
    tm = np.zeros((80, 80), np.float32)
    for s in range(64):
        for t in range(64):
            if s // 4 == t // 4 and s <= t:
                tm[s, t] = 1
    for s in range(64, 80):
        for t in range(64, 80):
            if s <= t:
                tm[s, t] = 1
    tmf = np.zeros((128, 8 * 80), np.float32)
    tmf[:80] = np.tile(tm, (1, 8))
    c["tmask"] = tmf
    rp = np.ones((128, 256), np.float32)
    rp[:, ::64] = 0
    c["reset_p"] = rp
    rt = np.ones((128, 80), np.float32)
    rt[:, 0:64:4] = 0
    rt[:, 64] = 0
    c["reset_t"] = rt
    rm = np.zeros((64, 16), np.float32)
    for b in range(16):
        rm[4 * b:4 * b + 4, b] = 1
    c["rowmask"] = rm
    oh = np.zeros((33, 384), np.float32)
    for i in range(384):
        dist = 255 - i
        if dist < 0:
            oh[32, i] = 1.0
        else:
            oh[_bucket(dist), i] += 1.0
            oh[31, i] -= 1.0
    c["oh"] = oh
    c["jflip"] = np.ascontiguousarray(np.eye(128, dtype=np.float32)[::-1])
    c["piota"] = np.arange(128, dtype=np.float32).reshape(128, 1)
    return c


CONST_SHAPES = {"ident_f": [128, 128], "hmask": [128, 512], "tmask": [128, 640], "reset_p": [128, 256],
                "reset_t": [128, 80], "rowmask": [64, 16], "oh": [33, 384], "jflip": [128, 128], "piota": [128, 1]}

WEIGHT_SHAPES = {
    "meta_tokens": [16, D], "norm_mix_w": [2, D], "norm_ffn_w": [2, D], "hg_w_in": [D, 4 * D],
    "hg_lower_bound": [2, D], "hg_norm_w": [1, D], "hg_w_out": [D, D], "at_w_qkv": [D, 3 * D],
    "at_lambda_q1": [1, 64], "at_lambda_k1": [1, 64], "at_lambda_q2": [1, 64], "at_lambda_k2": [1, 64],
    "at_subln_w": [1, 128], "at_w_out": [D, D], "rel_bias_table": [32, 8], "ff_w_up": [D, 2 * DFF],
    "ff_w_down": [DFF, D], "moe_w_router": [D, 8], "moe_b_router": [1, 8], "moe_w_up": [8 * D, 2 * DFFE],
    "moe_w_down": [8 * DFFE, D], "final_norm_w": [1, D],
}


def build(stage=99, dbg=False, nphys=N_PHYS, att_stop=0, do_sample=True, skip0=False, var=0):
    nc = bass.Bass("TRN2", target_bir_lowering=False)
    DUMPS = {}
    I = {}
    I["xp"] = nc.dram_tensor("xp", [2048, D], F32, kind="ExternalInput").ap()
    I["xs"] = nc.dram_tensor("xs", [64, D], F32, kind="ExternalInput").ap()
    I["state"] = nc.dram_tensor("state", [16, 8, 128, 128], F32, kind="ExternalInput").ap()
    I["cache_k"] = nc.dram_tensor("cache_k", [nphys * 128, D], F32, kind="ExternalInput").ap()
    I["cache_v"] = nc.dram_tensor("cache_v", [nphys * 128, D], F32, kind="ExternalInput").ap()
    I["ptab"] = nc.dram_tensor("ptab", [16, 16], I32, kind="ExternalInput").ap()
    for k, s in WEIGHT_SHAPES.items():
        I[k] = nc.dram_tensor(k, s, F32, kind="ExternalInput").ap()
    for k, s in CONST_SHAPES.items():
        I[k] = nc.dram_tensor(k, s, F32, kind="ExternalInput").ap()
    O = {}
    O["y_p"] = nc.dram_tensor("y_p", [2048, D], F32, kind="ExternalOutput").ap()
    O["y_s"] = nc.dram_tensor("y_s", [64, D], F32, kind="ExternalOutput").ap()
    O["st_p"] = nc.dram_tensor("st_p", [8, 128, 128], F32, kind="ExternalOutput").ap()
    O["st_s"] = nc.dram_tensor("st_s", [16, 8, 128, 128], F32, kind="ExternalOutput").ap()
    O["k_p"] = nc.dram_tensor("k_p", [2064, D], F32, kind="ExternalOutput").ap()
    O["v_p"] = nc.dram_tensor("v_p", [2064, D], F32, kind="ExternalOutput").ap()
    O["k_s"] = nc.dram_tensor("k_s", [64, D], F32, kind="ExternalOutput").ap()
    O["v_s"] = nc.dram_tensor("v_s", [64, D], F32, kind="ExternalOutput").ap()
    if dbg:
        O["dbg_x"] = nc.dram_tensor("dbg_x", [NT * 128, D], F32, kind="ExternalOutput").ap()
    obuf = Buf("dram_out")

    with ExitStack() as st:
        st.enter_context(nc.allow_low_precision(reason="bf16 matmul operands, fp32 accumulation"))
        S = Sched(nc, st)

        sbctr = [0]

        def sb(stack, name, shape, dt):
            sbctr[0] += 1
            return stack.enter_context(nc.sbuf_tensor(f"s{sbctr[0]}_{name}", shape, dt))

        X = sb(st, "X", [128, NT, D], F32)
        bX = [Buf(f"X{t}") for t in range(NT)]
        ident_f = sb(st, "ident_f", [128, 128], F32); b_idf = Buf("ident_f")
        ident_b = sb(st, "ident_b", [128, 128], BF16); b_idb = Buf("ident_b")
        NSLOT = 4
        ring = [sb(st, f"ring{i}", [128, 4096], BF16) for i in range(NSLOT)]
        bring = [Buf(f"ring{i}") for i in range(NSLOT)]
        ring_i = [0]
        PS = [st.enter_context(nc.psum_tensor(f"ps{i}", [128, 512], F32)) for i in range(7)]
        bPS = [Buf(f"ps{i}") for i in range(7)]
        PSB = st.enter_context(nc.psum_tensor("psb", [128, 1024], BF16)); bPSB = Buf("psb"); bPSBm = Buf("psbm"); bPSBp = [Buf("psbp0"), Buf("psbp1")]
        stat = sb(st, "stat", [128, 64], F32)
        bstat = Buf("stat", True)
        junk = sb(st, "junk", [128, D], F32); bjunk = Buf("junk")
        epsc = sb(st, "epsc", [128, 2], F32); b_epsc = Buf("epsc", True)
        wrow = sb(st, "wrow", [128, 2, D], F32)
        b_wrow = [Buf(f"wrow{i}") for i in range(2)]
        WR = {}
        WSRC = {"mix0": I["norm_mix_w"][0:1, :], "mix1": I["norm_mix_w"][1:2, :], "ffn0": I["norm_ffn_w"][0:1, :],
                "ffn1": I["norm_ffn_w"][1:2, :], "hgn": I["hg_norm_w"][0:1, :], "fin": I["final_norm_w"][0:1, :]}

        def load_wrow(nm, i):
            WR[nm] = i
            S.dma("sync", lambda h: h.dma_start(out=wrow[:, i, :], in_=WSRC[nm].partition_broadcast(128)), writes=[b_wrow[i]])

        def v_(f, reads=(), writes=(), inc=True):
            return S.op("vector", f, reads, writes, inc)

        def a_(f, reads=(), writes=(), inc=True):
            return S.op("scalar", f, reads, writes, inc)

        def p_(f, reads=(), writes=(), inc=True):
            return S.op("gpsimd", f, reads, writes, inc)

        def t_(f, reads=(), writes=(), inc=True):
            return S.op("tensor", f, reads, writes, inc)

        def dump(name, ap, shape, buf, dt=F32):
            if not dbg or name in DUMPS:
                return
            DUMPS[name] = nc.dram_tensor("dmp_" + name, shape, dt, kind="ExternalOutput").ap()
            S.dma("sync", lambda h: h.dma_start(out=DUMPS[name], in_=ap), reads=buf, writes=[obuf], sembuf=buf[0])

        v_(lambda h: h.memset(epsc[:, 0:1], EPS), [], [b_epsc])
        S.dma("sync", lambda h: h.dma_start(out=ident_f[:], in_=I["ident_f"][:, :]), writes=[b_idf])
        v_(lambda h: h.tensor_copy(ident_b[:], ident_f[:]), [b_idf], [b_idb])
        for t in range(16):
            S.dma("sync", lambda h: h.dma_start(out=X[:, t, :], in_=I["xp"][t * 128:(t + 1) * 128, :]), writes=[bX[t]])
        S.dma("sync", lambda h: h.dma_start(out=X[0:64, TAIL, :], in_=I["xs"][:, :]), writes=[bX[TAIL]])
        S.dma("sync", lambda h: h.dma_start(out=X[64:80, TAIL, :], in_=I["meta_tokens"][:, :]), writes=[bX[TAIL]])
        load_wrow("mix0", 0)
        load_wrow("hgn", 1)

        def rows(t):
            return 80 if t == TAIL else 128

        def wload(src_ap, nk, ncols, q="gpsimd"):
            i = ring_i[0] % NSLOT
            ring_i[0] += 1
            assert nk * ncols <= 4096
            if nk == 8:
                view = ring[i][:, :].rearrange("p (k n) -> p k n", k=8)[:, :, 0:ncols]
            else:
                view = ring[i][:, 0:nk * ncols].rearrange("p (k n) -> p k n", k=nk)
            S.dma(q, lambda h: h.dma_start(out=view, in_=src_ap.rearrange("(k p) n -> p k n", p=128)), writes=[bring[i]])
            return view, bring[i]

        def rmsnorm_to_hT(t, wname, hT_dst, b_hT, extra_fp32=None):
            r = rows(t)
            c0 = (t % 16)
            ss = stat[:r, 0:1]
            a_(lambda h: h.activation(junk[:r, :], X[:r, t, :], AF.Square, accum_out=ss), [bX[t]], [bjunk, bstat])
            a_(lambda h: h.activation(stat[:r, 2:3], ss, AF.Ln, scale=1.0 / D, bias=epsc[:r, 0:1]), [bstat, b_epsc], [bstat])
            a_(lambda h: h.activation(stat[:r, 3:4], stat[:r, 2:3], AF.Exp, scale=-0.5), [bstat], [bstat])
            hb = hbf[:r, :]
            v_(lambda h: h.scalar_tensor_tensor(hb, X[:r, t, :], stat[:r, 3:4], wrow[:r, WR[wname], :], ALU.mult, ALU.mult),
               [bX[t], bstat, b_wrow[WR[wname]]], [b_hbf])
            for k in range(8):
                t_(lambda h: h.transpose(PSB[:, k * 128:k * 128 + r], hbf[:r, k * 128:(k + 1) * 128], ident_b[:r, :r]),
                   [b_hbf, b_idb], [bPSB], inc=(k == 7))
            src = PSB[:, :].rearrange("p (k n) -> p k n", k=8)[:, :, 0:r]
            a_(lambda h: h.copy(hT_dst, src), [bPSB], [b_hT])

        hbf = sb(st, "hbf", [128, D], BF16); b_hbf = Buf("hbf")

        SEG = 2
        NSEGC = 4
        with ExitStack() as ph:
          if not skip0:
              lbt = sb(ph, "lbt", [128, 8, 8], F32); b_lbt = Buf("lbt", True)
              S.dma("sync", lambda h: h.dma_start(out=lbt[:, 0, :], in_=I["hg_lower_bound"][0:1, :].rearrange("o (k p) -> p (o k)", p=128), allow_slow_non_contiguous=True), writes=[b_lbt])
              S.dma("sync", lambda h: h.dma_start(out=lbt[:, 1, :], in_=I["hg_lower_bound"][1:2, :].rearrange("o (k p) -> p (o k)", p=128), allow_slow_non_contiguous=True), writes=[b_lbt])
              v_(lambda h: h.tensor_tensor(lbt[:, 2, :], lbt[:, 1, :], lbt[:, 0, :], ALU.subtract), [b_lbt], [b_lbt])
              a_(lambda h: h.activation(lbt[:, 3, :], lbt[:, 2, :], AF.Exp), [b_lbt], [b_lbt])
              v_(lambda h: h.tensor_scalar(lbt[:, 3, :], lbt[:, 3, :], 1.0, None, ALU.add), [b_lbt], [b_lbt])
              v_(lambda h: h.reciprocal(lbt[:, 4, :], lbt[:, 3, :]), [b_lbt], [b_lbt])
              v_(lambda h: h.tensor_scalar(lbt[:, 5, :], lbt[:, 4, :], -1.0, 1.0, ALU.mult, ALU.add), [b_lbt], [b_lbt])
              LB = lambda hd: lbt[:, 4, hd:hd + 1]
              OML = lambda hd: lbt[:, 5, hd:hd + 1]

              hmask = sb(ph, "hmask", [128, 512], F32); b_hmask = Buf("hmask")
              tmask = sb(ph, "tmask", [128, 640], F32); b_tmask = Buf("tmask")
              reset_p = sb(ph, "reset_p", [128, 256], F32); b_rp = Buf("reset_p")
              reset_t = sb(ph, "reset_t", [128, 80], F32); b_rt = Buf("reset_t")
              rowmask = sb(ph, "rowmask", [64, 16], F32); b_rm = Buf("rowmask")
              for tl, bf, nm in ((hmask, b_hmask, "hmask"), (tmask, b_tmask, "tmask"), (reset_p, b_rp, "reset_p"),
                                 (reset_t, b_rt, "reset_t"), (rowmask, b_rm, "rowmask")):
                  S.dma("sync", lambda h: h.dma_start(out=tl[:], in_=I[nm][:, :]), writes=[bf])

              NS = SEG * 128
              hT = sb(ph, "hT", [128, 8, NS], BF16); b_hT = Buf("hT")
              qT = sb(ph, "qT", [128, 8, NS], BF16); b_qT = [Buf(f"qT{h}") for h in range(8)]
              kT = sb(ph, "kT", [128, 8, NS], BF16); b_kT = [Buf(f"kT{h}") for h in range(8)]
              vtok = sb(ph, "vtok", [128, SEG, D], BF16); b_vtok = [Buf(f"vtok{i}") for i in range(SEG)]
              sgt = sb(ph, "sgt", [128, SEG, D], BF16); b_sgt = [Buf(f"sgt{i}") for i in range(SEG)]
              ktok = sb(ph, "ktok", [128, SEG, D], BF16); b_ktok = [Buf(f"ktok{i}") for i in range(SEG)]
              f1 = sb(ph, "f1", [128, 512], F32); b_f1 = Buf("f1")
              f2 = sb(ph, "f2", [128, 512], F32); b_f2 = Buf("f2")
              f3 = sb(ph, "f3", [128, 512], F32); b_f3 = Buf("f3")
              Gt = sb(ph, "Gt", [128, NS], F32); b_G = Buf("Gt")
              csc = sb(ph, "csc", [128, 8, 8, 4], F32); b_csc = Buf("csc", True)
              Sst = sb(ph, "Sst", [128, 8, 128], F32); b_S = Buf("Sst")
              Sp = sb(ph, "Sp", [128, 8, 128], BF16); b_Sp = Buf("Sp")
              AT = sb(ph, "AT", [128, 8, 80], BF16); b_AT = Buf("AT")
              og = sb(ph, "og", [128, D], F32); b_og = Buf("og")
              ogb = sb(ph, "ogb", [128, D], BF16); b_ogb = Buf("ogb")
              ogT = sb(ph, "ogT", [128, 8, NS], BF16); b_ogT = [Buf(f"ogT{i}") for i in range(SEG)]
              v_(lambda h: h.memset(Sst[:], 0.0), [], [b_S])
              v_(lambda h: h.memset(Sp[:], 0.0), [], [b_Sp])

              W = I["hg_w_in"]

              def hgrn_segment(seg):
                  tail = seg == "tail"
                  tiles = [TAIL] if tail else [SEG * seg + i for i in range(SEG)]
                  N = 80 if tail else NS
                  for i, t in enumerate(tiles):
                      r = rows(t)
                      rmsnorm_to_hT(t, "mix0", hT[:, :, i * 128:i * 128 + r], b_hT)
                  for half in range(2):
                      wq, bwq = wload(W[:, half * 512:(half + 1) * 512], 8, 512)
                      wf, bwf = wload(W[:, 1024 + half * 512:1024 + (half + 1) * 512], 8, 512)
                      for hh in range(4):
                          hd = half * 4 + hh
                          pq, bpq, pf, bpf = PS[0], bPS[0], PS[1], bPS[1]
                          for k in range(8):
                              t_(lambda h: h.matmul(pq[:, :N], wq[:, k, hh * 128:(hh + 1) * 128], hT[:, k, :N], start=(k == 0), stop=(k == 7)),
                                 [bwq, b_hT], [bpq], inc=(k == 7))
                          for k in range(8):
                              t_(lambda h: h.matmul(pf[:, :N], wf[:, k, hh * 128:(hh + 1) * 128], hT[:, k, :N], start=(k == 0), stop=(k == 7)),
                                 [bwf, b_hT], [bpf], inc=(k == 7))
                          a_(lambda h: h.activation(f1[:, :N], pq[:, :N], AF.Exp, scale=-1.0), [bpq], [b_f1])
                          v_(lambda h: h.tensor_scalar(f1[:, :N], f1[:, :N], 1.0, None, ALU.add), [b_f1], [b_f1])
                          v_(lambda h: h.reciprocal(f1[:, :N], f1[:, :N]), [b_f1], [b_f1])
                          v_(lambda h: h.tensor_tensor(f1[:, :N], pq[:, :N], f1[:, :N], ALU.mult), [bpq, b_f1], [b_f1])
                          a_(lambda h: h.activation(f2[:, :N], pf[:, :N], AF.Exp, scale=-1.0), [bpf], [b_f2])
                          v_(lambda h: h.tensor_scalar(f2[:, :N], f2[:, :N], 1.0, None, ALU.add), [b_f2], [b_f2])
                          v_(lambda h: h.reciprocal(f2[:, :N], f2[:, :N]), [b_f2], [b_f2])
                          v_(lambda h: h.tensor_scalar(f2[:, :N], f2[:, :N], OML(hd), LB(hd), ALU.mult, ALU.add), [b_f2, b_lbt], [b_f2])
                          a_(lambda h: h.activation(f3[:, :N], f2[:, :N], AF.Ln), [b_f2], [b_f3])
                          v_(lambda h: h.tensor_scalar(f2[:, :N], f2[:, :N], -1.0, 1.0, ALU.mult, ALU.add), [b_f2], [b_f2])
                          rs = reset_t if tail else reset_p
                          v_(lambda h: h.tensor_tensor_scan(Gt[:, :N], rs[:, :N], f3[:, :N], 0.0, ALU.mult, ALU.add),
                             [b_f3, b_rt if tail else b_rp], [b_G])
                          if not tail:
                              G3 = Gt[:, :].rearrange("p (c n) -> p c n", n=64)
                              a_(lambda h: h.activation(csc[:, hd, 0:NSEGC, 0], G3[:, :, 31], AF.Exp), [b_G], [b_csc])
                              a_(lambda h: h.activation(csc[:, hd, 0:NSEGC, 1], G3[:, :, 63], AF.Exp), [b_G], [b_csc])
                              v_(lambda h: h.tensor_tensor(csc[:, hd, 0:NSEGC, 3], G3[:, :, 63], G3[:, :, 31], ALU.subtract), [b_G], [b_csc])
                              a_(lambda h: h.activation(csc[:, hd, 0:NSEGC, 2], csc[:, hd, 0:NSEGC, 3], AF.Exp), [b_csc], [b_csc])
                              f33 = f3[:, 0:NS].rearrange("p (c n) -> p c n", n=64)
                              v_(lambda h: h.tensor_tensor(f33, G3, G3[:, :, 31:32].to_broadcast([128, NSEGC, 64]), ALU.subtract), [b_G], [b_f3])
                              gc, b_gc = f3, b_f3
                          else:
                              cflat = csc[:, hd, :, :].rearrange("p a b -> p (a b)")
                              a_(lambda h: h.activation(cflat[:, 0:16], Gt[:, 3:64:4], AF.Exp), [b_G], [b_csc])
                              a_(lambda h: h.activation(cflat[:, 16:17], Gt[:, 79:80], AF.Exp), [b_G], [b_csc])
                              gc, b_gc = Gt, b_G
                          a_(lambda h: h.activation(junk[:, :N], gc[:, :N], AF.Exp), [b_gc], [bjunk])
                          v_(lambda h: h.tensor_tensor(qT[:, hd, :N], f1[:, :N], junk[:, :N], ALU.mult), [b_f1, bjunk], [b_qT[hd]])
                          a_(lambda h: h.activation(junk[:, 512:512 + N], gc[:, :N], AF.Exp, scale=-1.0), [b_gc], [bjunk])
                          v_(lambda h: h.tensor_tensor(kT[:, hd, :N], f2[:, :N], junk[:, 512:512 + N], ALU.mult), [b_f2, bjunk], [b_kT[hd]])
                  for half in range(2):
                      wi, bwi = wload(W[:, 2048 + half * 512:2048 + (half + 1) * 512], 8, 512)
                      wg, bwg = wload(W[:, 3072 + half * 512:3072 + (half + 1) * 512], 8, 512)
                      for i, t in enumerate(tiles):
                          r = rows(t)
                          pv, bpv, pg, bpg = PS[2], bPS[2], PS[3], bPS[3]
                          for k in range(8):
                              t_(lambda h: h.matmul(pv[:r, :], hT[:, k, i * 128:i * 128 + r], wi[:, k, :], start=(k == 0), stop=(k == 7)),
                                 [bwi, b_hT], [bpv], inc=(k == 7))
                          for k in range(8):
                              t_(lambda h: h.matmul(pg[:r, :], hT[:, k, i * 128:i * 128 + r], wg[:, k, :], start=(k == 0), stop=(k == 7)),
                                 [bwg, b_hT], [bpg], inc=(k == 7))
                          a_(lambda h: h.copy(vtok[:r, i, half * 512:(half + 1) * 512], pv[:r, :]), [bpv], [b_vtok[i]])
                          a_(lambda h: h.activation(f3[:r, :], pg[:r, :], AF.Exp, scale=-1.0), [bpg], [b_f3])
                          v_(lambda h: h.tensor_scalar(f3[:r, :], f3[:r, :], 1.0, None, ALU.add), [b_f3], [b_f3])
                          v_(lambda h: h.reciprocal(sgt[:r, i, half * 512:(half + 1) * 512], f3[:r, :]), [b_f3], [b_sgt[i]])
                  for i, t in enumerate(tiles):
                      r = rows(t)
                      for hd in range(8):
                          t_(lambda h: h.transpose(PSB[:r, hd * 128:(hd + 1) * 128], kT[:, hd, i * 128:i * 128 + r], ident_b[:, :]),
                             [b_kT[hd], b_idb], [bPSB], inc=(hd == 7))
                      a_(lambda h: h.copy(ktok[:r, i, :], PSB[:r, :]), [bPSB], [b_ktok[i]])
                  tg = "t" if tail else f"s{seg}"
                  if tail or seg == 0:
                      dump(f"hT_{tg}", hT[:, :, :], [128, 8, NS], [b_hT], BF16)
                      dump(f"qT_{tg}", qT[:, :, :], [128, 8, NS], b_qT, BF16)
                      dump(f"kT_{tg}", kT[:, :, :], [128, 8, NS], b_kT, BF16)
                      dump(f"vtok_{tg}", vtok[:, :, :], [128, SEG, D], b_vtok, BF16)
                      dump(f"sgt_{tg}", sgt[:, :, :], [128, SEG, D], b_sgt, BF16)
                      dump(f"ktok_{tg}", ktok[:, :, :], [128, SEG, D], b_ktok, BF16)
                      dump(f"csc_{tg}", csc[:, :, :, :], [128, 8, 8, 4], [b_csc])
                      dump(f"G_{tg}", Gt[:, :], [128, NS], [b_G])
                  return tiles

              def gate_norm(t, i, po_list):
                  r = rows(t)
                  for hf in range(2):
                      v_(lambda h: h.tensor_tensor(og[:r, hf * 512:(hf + 1) * 512], po_list[hf][0][:r, :], sgt[:r, i, hf * 512:(hf + 1) * 512], ALU.mult),
                         [po_list[hf][1], b_sgt[i]], [b_og])
                  ss = stat[:r, 8:9]
                  dump(f"og_{t}", og[:, :], [128, D], [b_og])
                  a_(lambda h: h.activation(junk[:r, :], og[:r, :], AF.Square, accum_out=ss), [b_og], [bjunk, bstat])
                  a_(lambda h: h.activation(stat[:r, 10:11], ss, AF.Ln, scale=1.0 / D, bias=epsc[:r, 0:1]), [bstat, b_epsc], [bstat])
                  a_(lambda h: h.activation(stat[:r, 11:12], stat[:r, 10:11], AF.Exp, scale=-0.5), [bstat], [bstat])
                  v_(lambda h: h.scalar_tensor_tensor(ogb[:r, :], og[:r, :], stat[:r, 11:12], wrow[:r, WR["hgn"], :], ALU.mult, ALU.mult),
                     [b_og, bstat, b_wrow[WR["hgn"]]], [b_ogb])
                  for k in range(8):
                      t_(lambda h: h.transpose(PSB[:, k * 128:k * 128 + r], ogb[:r, k * 128:(k + 1) * 128], ident_b[:r, :r]),
                         [b_ogb, b_idb], [bPSB], inc=(k == 7))
                  a_(lambda h: h.copy(ogT[:, :, i * 128:i * 128 + r], PSB[:, :].rearrange("p (k n) -> p k n", k=8)[:, :, 0:r]), [bPSB], [b_ogT[i]])

              def out_proj(tiles):
                  for hf in range(2):
                      wo, bwo = wload(I["hg_w_out"][:, hf * 512:(hf + 1) * 512], 8, 512)
                      for i, t in enumerate(tiles):
                          r = rows(t)
                          pz, bpz = PS[4 + (i % 2)], bPS[4 + (i % 2)]
                          for k in range(8):
                              t_(lambda h: h.matmul(pz[:r, :], ogT[:, k, i * 128:i * 128 + r], wo[:, k, :], start=(k == 0), stop=(k == 7)),
                                 [b_ogT[i], bwo], [bpz], inc=(k == 7))
                          v_(lambda h: h.tensor_tensor(X[:r, t, hf * 512:(hf + 1) * 512], X[:r, t, hf * 512:(hf + 1) * 512], pz[:r, :], ALU.add),
                             [bX[t], bpz], [bX[t]])

              S.force_same = True
              hgrn_segment("tail")
              with ExitStack() as tl:
                  S0b = sb(tl, "S0b", [128, 2, 16, 128], BF16); b_S0b = [Buf("S0b0"), Buf("S0b1")]
                  Z = sb(tl, "Z", [128, 2, 16, 80], BF16); b_Z = [Buf("Z0"), Buf("Z1")]
                  KM = sb(tl, "KM", [64, D], BF16); b_KM = Buf("KM")
                  S0f = sb(tl, "S0f", [128, 1, D], F32); b_S0f = [Buf("S0f0")]
                  Sn = og[:, :].rearrange("p (o n) -> p o n", o=1); b_Sn = [b_og]
                  v_(lambda h: h.memset(Z[:], 0.0), [], b_Z)
                  for hd in range(8):
                      pa, bpa = (PS[0], bPS[0]) if hd < 4 else (PS[1], bPS[1])
                      t_(lambda h: h.matmul(pa[:80, (hd % 4) * 80:(hd % 4) * 80 + 80], kT[:, hd, 0:80], qT[:, hd, 0:80], start=True, stop=True),
                         [b_kT[hd], b_qT[hd]], [bpa], inc=(hd % 4 == 3))
                  v_(lambda h: h.tensor_tensor(AT[:80, 0:4, :], PS[0][:80, 0:320].rearrange("p (a b) -> p a b", a=4),
                                               tmask[:80, 0:320].rearrange("p (a b) -> p a b", a=4), ALU.mult), [bPS[0], b_tmask], [b_AT])
                  v_(lambda h: h.tensor_tensor(AT[:80, 4:8, :], PS[1][:80, 0:320].rearrange("p (a b) -> p a b", a=4),
                                               tmask[:80, 320:640].rearrange("p (a b) -> p a b", a=4), ALU.mult), [bPS[1], b_tmask], [b_AT])
                  for hd in range(8):
                      j = hd % 2
                      S.dma("gpsimd", lambda h: h.dma_start(out=S0b[:, j, :, :], in_=I["state"][:, hd, :, :].rearrange("b p v -> p b v")), writes=[b_S0b[j]])
                      for b in range(16):
                          v_(lambda h: h.tensor_copy(Z[:, j, b, 4 * b:4 * b + 4], qT[:, hd, 4 * b:4 * b + 4]), [b_qT[hd]], [b_Z[j]])
                      po, bpo = (PS[2], bPS[2]) if hd < 4 else (PS[3], bPS[3])
                      c0 = (hd % 4) * 128
                      t_(lambda h: h.matmul(po[:80, c0:c0 + 128], AT[:80, hd, :], vtok[:80, 0, hd * 128:(hd + 1) * 128], start=True, stop=False),
                         [b_AT, b_vtok[0]], [bpo], inc=False)
                      for b in range(16):
                          t_(lambda h: h.matmul(po[:80, c0:c0 + 128], Z[:, j, b, :], S0b[:, j, b, :], start=False, stop=(b == 15)),
                             [b_Z[j], b_S0b[j]], [bpo], inc=(b == 15))
                  gate_norm(TAIL, 0, [(PS[2], bPS[2]), (PS[3], bPS[3])])
                  out_proj([TAIL])
                  cflat = lambda hd: csc[:, hd, :, :].rearrange("p a b -> p (a b)")
                  for b in range(16):
                      j = 0
                      S.dma("sync", lambda h: h.dma_start(out=S0f[:, j, :].rearrange("p (hh v) -> p hh v", hh=8),
                                                          in_=I["state"][b].rearrange("hh p v -> p hh v")), writes=[b_S0f[j]])
                      v_(lambda h: h.tensor_scalar(KM[:, :], ktok[0:64, 0, :], rowmask[:, b:b + 1], None, ALU.mult), [b_ktok[0], b_rm], [b_KM])
                      for hd in range(8):
                          pm, bpm = (PS[4], bPS[4]) if hd < 4 else (PS[5], bPS[5])
                          c0 = (hd % 4) * 128
                          t_(lambda h: h.matmul(pm[:, c0:c0 + 128], KM[:, hd * 128:(hd + 1) * 128], vtok[0:64, 0, hd * 128:(hd + 1) * 128], start=True, stop=True),
                             [b_KM, b_vtok[0]], [bpm], inc=(hd % 4 == 3))
                      for hd in range(8):
                          pm, bpm = (PS[4], bPS[4]) if hd < 4 else (PS[5], bPS[5])
                          c0 = (hd % 4) * 128
                          v_(lambda h: h.tensor_tensor(Sn[:, j, hd * 128:(hd + 1) * 128], pm[:, c0:c0 + 128], S0f[:, j, hd * 128:(hd + 1) * 128], ALU.add),
                             [bpm, b_S0f[j]], [b_Sn[j]])
                          v_(lambda h: h.tensor_scalar(Sn[:, j, hd * 128:(hd + 1) * 128], Sn[:, j, hd * 128:(hd + 1) * 128], cflat(hd)[:, b:b + 1], None, ALU.mult),
                             [b_csc], [b_Sn[j]])
                      S.dma("sync", lambda h: h.dma_start(out=O["st_s"][b].rearrange("hh p v -> p hh v"),
                                                          in_=Sn[:, j, :].rearrange("p (hh v) -> p hh v", hh=8)), reads=[b_Sn[j]], writes=[obuf], sembuf=b_Sn[j])
                  for hd in range(8):
                      pm, bpm = (PS[4], bPS[4]) if hd < 4 else (PS[5], bPS[5])
                      c0 = (hd % 4) * 128
                      t_(lambda h: h.matmul(pm[:, c0:c0 + 128], ktok[64:80, 0, hd * 128:(hd + 1) * 128], vtok[64:80, 0, hd * 128:(hd + 1) * 128], start=True, stop=True),
                         [b_ktok[0], b_vtok[0]], [bpm], inc=(hd % 4 == 3))
                  for hd in range(8):
                      pm, bpm = (PS[4], bPS[4]) if hd < 4 else (PS[5], bPS[5])
                      c0 = (hd % 4) * 128
                      v_(lambda h: h.tensor_scalar(Sst[:, hd, :], pm[:, c0:c0 + 128], cflat(hd)[:, 16:17], None, ALU.mult), [bpm, b_csc], [b_S])
                  S.barrier()
              S.force_same = False
              for seg in range(16 // SEG):
                  tiles = hgrn_segment(seg)
                  for ci in range(NSEGC):
                      i = ci // 2
                      t = tiles[i]
                      p0 = 64 * (ci % 2)
                      cs = slice(ci * 64, ci * 64 + 64)
                      for hd in range(8):
                          v_(lambda h: h.tensor_scalar(Sp[:, hd, :], Sst[:, hd, :], csc[:, hd, ci, 0:1], None, ALU.mult), [b_S, b_csc], [b_Sp])
                      pa, bpa = PS[0], bPS[0]
                      for hd in range(8):
                          t_(lambda h: h.matmul(pa[p0:p0 + 64, hd * 64:(hd + 1) * 64], kT[:, hd, cs], qT[:, hd, cs], start=True, stop=True),
                             [b_kT[hd], b_qT[hd]], [bpa], inc=(hd == 7))
                      v_(lambda h: h.tensor_tensor(AT[p0:p0 + 64, :, 0:64], pa[p0:p0 + 64, :].rearrange("p (a b) -> p a b", a=8),
                                                   hmask[p0:p0 + 64, :].rearrange("p (a b) -> p a b", a=8), ALU.mult), [bpa, b_hmask], [b_AT])
                      for hd in range(8):
                          po, bpo = (PS[2], bPS[2]) if hd < 4 else (PS[3], bPS[3])
                          c0 = (hd % 4) * 128
                          t_(lambda h: h.matmul(po[p0:p0 + 64, c0:c0 + 128], AT[p0:p0 + 64, hd, 0:64], vtok[p0:p0 + 64, i, hd * 128:(hd + 1) * 128], start=True, stop=False),
                             [b_AT, b_vtok[i]], [bpo], inc=False)
                          t_(lambda h: h.matmul(po[p0:p0 + 64, c0:c0 + 128], qT[:, hd, cs], Sp[:, hd, :], start=False, stop=True),
                             [b_qT[hd], b_Sp], [bpo], inc=(hd % 4 == 3))
                      for hd in range(8):
                          pm, bpm = (PS[4], bPS[4]) if hd < 4 else (PS[5], bPS[5])
                          c0 = (hd % 4) * 128
                          t_(lambda h: h.matmul(pm[:, c0:c0 + 128], ktok[p0:p0 + 64, i, hd * 128:(hd + 1) * 128], vtok[p0:p0 + 64, i, hd * 128:(hd + 1) * 128], start=True, stop=True),
                             [b_ktok[i], b_vtok[i]], [bpm], inc=(hd % 4 == 3))
                      for hd in range(8):
                          pm, bpm = (PS[4], bPS[4]) if hd < 4 else (PS[5], bPS[5])
                          c0 = (hd % 4) * 128
                          v_(lambda h: h.tensor_scalar(Sst[:, hd, :], Sst[:, hd, :], csc[:, hd, ci, 1:2], None, ALU.mult), [b_csc], [b_S])
                          v_(lambda h: h.scalar_tensor_tensor(Sst[:, hd, :], pm[:, c0:c0 + 128], csc[:, hd, ci, 2:3], Sst[:, hd, :], ALU.mult, ALU.add),
                             [bpm, b_csc], [b_S])
                      if ci % 2 == 1:
                          gate_norm(t, i, [(PS[2], bPS[2]), (PS[3], bPS[3])])
                  out_proj(tiles)
              S.dma("sync", lambda h: h.dma_start(out=O["st_p"].rearrange("hh p v -> p hh v"), in_=Sst[:, :, :]), reads=[b_S], writes=[obuf], sembuf=b_S)
              S.barrier()

        TB = [(0, 512), (512, 512), (1024, 512), (1536, 512), (2048, 80)]

        def tcols(t):
            return slice(t * 128, t * 128 + rows(t))

        def norm_all(wname, hTf, b_hTf):
            for t in range(NT):
                rmsnorm_to_hT(t, wname, hTf[:, :, tcols(t)], b_hTf[t])

        def ffn(w_up, w_dn, F, row0_up, row0_dn, hTf, b_hTf, act, b_act, gate_ap, b_gate, ftmp, b_ftmp, gctr):
            nch = F // 128
            c0 = 0
            while c0 < nch:
                gs = min(4, nch - c0)
                gi = gctr[0] % 2
                gctr[0] += 1
                wa, bwa = wload(w_up[row0_up:row0_up + D, c0 * 128:(c0 + gs) * 128], 8, gs * 128)
                wb, bwb = wload(w_up[row0_up:row0_up + D, F + c0 * 128:F + (c0 + gs) * 128], 8, gs * 128)
                wd, bwd = wload(w_dn[row0_dn + c0 * 128:row0_dn + (c0 + gs) * 128, :], gs, D)
                for j in range(gs):
                    for bi, (cb, n) in enumerate(TB):
                        x = (j * len(TB) + bi) % 2
                        pa, bpa, pb, bpb = PS[2 * x], bPS[2 * x], PS[2 * x + 1], bPS[2 * x + 1]
                        rd = b_hTf[cb // 128:cb // 128 + (n + 127) // 128]
                        for k in range(8):
                            t_(lambda h: h.matmul(pa[:, :n], wa[:, k, j * 128:(j + 1) * 128], hTf[:, k, cb:cb + n], start=(k == 0), stop=(k == 7)),
                               [bwa] + rd, [bpa], inc=(k == 7))
                        for k in range(8):
                            t_(lambda h: h.matmul(pb[:, :n], wb[:, k, j * 128:(j + 1) * 128], hTf[:, k, cb:cb + n], start=(k == 0), stop=(k == 7)),
                               [bwb] + rd, [bpb], inc=(k == 7))
                        a_(lambda h: h.activation(ftmp[:, x, :n], pa[:, :n], AF.Silu), [bpa], [b_ftmp[x]])
                        v_(lambda h: h.tensor_tensor(act[:, gi, j, cb:cb + n], ftmp[:, x, :n], pb[:, :n], ALU.mult), [b_ftmp[x], bpb], [b_act[gi]])
                for t in range(NT):
                    r = rows(t)
                    for hf in range(2):
                        pz, bpz = PS[4 + hf], bPS[4 + hf]
                        for j in range(gs):
                            t_(lambda h: h.matmul(pz[:r, :], act[:, gi, j, tcols(t)], wd[:, j, hf * 512:(hf + 1) * 512], start=(j == 0), stop=(j == gs - 1)),
                               [b_act[gi], bwd], [bpz], inc=(j == gs - 1))
                        xs_ = X[:r, t, hf * 512:(hf + 1) * 512]
                        if gate_ap is None:
                            v_(lambda h: h.tensor_tensor(xs_, xs_, pz[:r, :], ALU.add), [bX[t], bpz], [bX[t]])
                        else:
                            v_(lambda h: h.scalar_tensor_tensor(xs_, pz[:r, :], gate_ap(t, r), xs_, ALU.mult, ALU.add), [bX[t], bpz, b_gate], [bX[t]])
                c0 += gs

        def ffn_phase(layer):
            with ExitStack() as ph:
                hTf = sb(ph, "hTf", [128, 8, NTOK], BF16); b_hTf = [Buf(f"hTf{t}") for t in range(NT)]
                act = sb(ph, "act", [128, 2, 4, NTOK], BF16); b_act = [Buf("act0"), Buf("act1")]
                ftmp = sb(ph, "ftmp", [128, 2, 512], F32); b_ftmp = [Buf("ftmp0"), Buf("ftmp1")]
                gctr = [0]
                load_wrow("ffn0" if layer == 0 else "ffn1", 0)
                norm_all("ffn0" if layer == 0 else "ffn1", hTf, b_hTf)
                if layer == 0:
                    ffn(I["ff_w_up"], I["ff_w_down"], DFF, 0, 0, hTf, b_hTf, act, b_act, None, None, ftmp, b_ftmp, gctr)
                else:
                    gates = sb(ph, "gates", [128, NT, 8], F32); b_gates = Buf("gates", True)
                    rt = sb(ph, "rt", [128, 8, 8], F32); b_rt_ = Buf("rt", True)
                    wr = sb(ph, "wr", [128, 8, 256], BF16); b_wr = Buf("wr")
                    v_(lambda h: h.memset(wr[:], 0.0), [], [b_wr])
                    brow = sb(ph, "brow", [128, 8], F32); b_brow = Buf("brow")
                    S.dma("gpsimd", lambda h: h.dma_start(out=wr[:, :, 0:8], in_=I["moe_w_router"].rearrange("(k p) n -> p k n", p=128)), writes=[b_wr])
                    S.dma("sync", lambda h: h.dma_start(out=brow[:], in_=I["moe_b_router"][0:1, :].partition_broadcast(128)), writes=[b_brow])
                    for t in range(NT):
                        r = rows(t)
                        pl, bpl = PS[5], bPS[5]
                        for k in range(8):
                            t_(lambda h: h.matmul(pl[:r, 0:256], hTf[:, k, tcols(t)], wr[:, k, :], start=(k == 0), stop=(k == 7)), [b_hTf[t], b_wr], [bpl], inc=(k == 7))
                        lg = rt[:r, 0, :]
                        v_(lambda h: h.tensor_tensor(lg, pl[:r, 0:8], brow[:r, :], ALU.add), [bpl, b_brow], [b_rt_])
                        v_(lambda h: h.max(rt[:r, 1, :], lg), [b_rt_], [b_rt_])
                        v_(lambda h: h.tensor_tensor(rt[:r, 2, 0:1], rt[:r, 1, 0:1], rt[:r, 1, 1:2], ALU.subtract), [b_rt_], [b_rt_])
                        a_(lambda h: h.activation(rt[:r, 2, 1:2], rt[:r, 2, 0:1], AF.Exp), [b_rt_], [b_rt_])
                        v_(lambda h: h.tensor_scalar(rt[:r, 2, 1:2], rt[:r, 2, 1:2], 1.0, None, ALU.add), [b_rt_], [b_rt_])
                        v_(lambda h: h.reciprocal(rt[:r, 2, 2:3], rt[:r, 2, 1:2]), [b_rt_], [b_rt_])
                        v_(lambda h: h.tensor_scalar(rt[:r, 2, 3:4], rt[:r, 2, 2:3], -1.0, 1.0, ALU.mult, ALU.add), [b_rt_], [b_rt_])
                        v_(lambda h: h.tensor_scalar(rt[:r, 3, :], lg, rt[:r, 1, 0:1], rt[:r, 2, 3:4], ALU.is_equal, ALU.mult), [b_rt_], [b_rt_])
                        v_(lambda h: h.tensor_scalar(rt[:r, 4, :], lg, rt[:r, 1, 1:2], rt[:r, 2, 2:3], ALU.is_equal, ALU.mult), [b_rt_], [b_rt_])
                        v_(lambda h: h.tensor_tensor(gates[:r, t, :], rt[:r, 3, :], rt[:r, 4, :], ALU.add), [b_rt_], [b_gates])
                    for e in range(8):
                        ffn(I["moe_w_up"], I["moe_w_down"], DFFE, e * D, e * DFFE, hTf, b_hTf, act, b_act,
                            (lambda t, r, e=e: gates[:r, t, e:e + 1]), b_gates, ftmp, b_ftmp, gctr)
                S.barrier()

        LAMBDA_INIT = 0.8 - 0.6 * float(np.exp(-0.3 * 1))
        NEGM = -30000.0
        wscr = nc.dram_tensor("wscr", [8, 384], F32, kind="Internal").ap()
        w2scr = nc.dram_tensor("w2scr", [384, 8], F32, kind="Internal").ap()
        b_wscr = Buf("wscr"); b_w2scr = Buf("w2scr")

        def dram_ap(base, offset, pat):
            return bass.AP(tensor=base.tensor, offset=offset, ap=pat)

        def sample_pass(hTf, qTt, b_qTt, kTt, b_kTt, b_small, NLAM, swb, b_swb, b_vp):
            S.force_same = True
            with ExitStack() as sp:
                KTall = hTf[:, :, :].rearrange("p a b -> p (a b)")[:, 0:16384].rearrange("p (a h k) -> p a h k", a=16, h=8)
                b_KT = [Buf(f"KT{p}") for p in range(16)]
                Vpg = lambda pg: ring[pg // 4][:, (pg % 4) * 1024:(pg % 4 + 1) * 1024]
                b_V = [bring[pg // 4] for pg in range(16)]
                Kraw = sb(sp, "Kraw", [128, 2, D], BF16); b_Kraw = [Buf("Kraw0"), Buf("Kraw1")]
                scs2 = [sb(sp, f"scs{i}", [8, 2052], F32) for i in range(2)]; b_scs2 = [Buf("scs0"), Buf("scs1")]
                Pbs2 = [sb(sp, f"Pbs{i}", [8, 2052], BF16) for i in range(2)]; b_Pbs2 = [Buf("Pbs0"), Buf("Pbs1")]
                PTs2 = [sb(sp, f"PTs{i}", [128, 16, 8], BF16) for i in range(2)]; b_PTs2 = [Buf("PTs0"), Buf("PTs1")]
                PTn2 = [sb(sp, f"PTn{i}", [4, 8], BF16) for i in range(2)]; b_PTn2 = [Buf("PTn0"), Buf("PTn1")]
                Qb2 = [sb(sp, f"Qb{i}", [128, 8], BF16) for i in range(2)]; b_Qb2 = [Buf("Qb0"), Buf("Qb1")]
                Bs = sb(sp, "Bs", [8, 8, 132], F32); b_Bs = Buf("Bs")
                vnew = sb(sp, "vnew", [4, D], BF16); b_vnew = Buf("vnew")
                oTs = sb(sp, "oTs", [128, 8, 64], BF16); b_oTs = Buf("oTs")
                o1s = sb(sp, "o1s", [4, 128], F32); b_o1s = Buf("o1s")
                ods = sb(sp, "ods", [4, 128], F32); b_ods = Buf("ods")
                osbs = sb(sp, "osbs", [4, 128], BF16); b_osbs = Buf("osbs")
                st32 = [sb(sp, f"st3{i}", [8, 16], F32) for i in range(2)]; b_st32 = [Buf("st30"), Buf("st31")]
                pti = sb(sp, "pti", [128, 256], I32); b_pti = Buf("pti")
                ptf = sb(sp, "ptf", [128, 256], F32); b_ptf = Buf("ptf")
                idx = sb(sp, "idx", [128, 256], I32); b_idx = Buf("idx")
                pio = sb(sp, "pio", [128, 1], F32); b_pio = Buf("pio")
                S.dma("sync", lambda h: h.dma_start(out=pio[:], in_=I["piota"][:, :]), writes=[b_pio])
                S.dma("sync", lambda h: h.dma_start(out=pti[:], in_=I["ptab"].rearrange("b (o p) -> o (b p)", o=1).partition_broadcast(128)), writes=[b_pti])
                v_(lambda h: h.tensor_copy(ptf[:], pti[:]), [b_pti], [b_ptf])
                v_(lambda h: h.tensor_scalar(ptf[:], ptf[:], 128.0, pio[:, 0:1], ALU.mult, ALU.add), [b_ptf, b_pio], [b_ptf])
                v_(lambda h: h.tensor_copy(idx[:], ptf[:]), [b_ptf], [b_idx])
                for m in range(2):
                    for q in range(4):
                        rw = m * 4 + q
                        S.dma("sync", lambda h: h.dma_start(out=Bs[rw:rw + 1, :, 0:128], in_=dram_ap(wscr, 127 - q, [[1, 1], [384, 8], [1, 128]])), reads=[b_wscr], writes=[b_Bs])
                        S.dma("sync", lambda h: h.dma_start(out=Bs[rw:rw + 1, :, 128:132], in_=dram_ap(wscr, 255 - q, [[1, 1], [384, 8], [1, 4]])), reads=[b_wscr], writes=[b_Bs])
                for b in range(16):
                    for pg in range(16):
                        j = pg % 2
                        c = b * 16 + pg
                        S.dma("gpsimd", lambda h: h.indirect_dma_start(out=Kraw[:, j, :], out_offset=None, in_=I["cache_k"][:, :],
                                                                       in_offset=bass.IndirectOffsetOnAxis(ap=idx[:, c:c + 1], axis=0)), reads=[b_idx], writes=[b_Kraw[j]])
                        S.dma("gpsimd", lambda h: h.indirect_dma_start(out=Vpg(pg), out_offset=None, in_=I["cache_v"][:, :],
                                                                       in_offset=bass.IndirectOffsetOnAxis(ap=idx[:, c:c + 1], axis=0)), reads=[b_idx], writes=[b_V[pg]])
                        for hd in range(8):
                            t_(lambda h: h.transpose(PSB[:, hd * 128:(hd + 1) * 128], Kraw[:, j, hd * 128:(hd + 1) * 128], ident_b[:, :]), [b_Kraw[j], b_idb], [bPSB, bPSBp[0], bPSBp[1]], inc=(hd == 7))
                        a_(lambda h: h.copy(KTall[:, pg, :, :], PSB[:, :].rearrange("p (h k) -> p h k", h=8)), [bPSB], [b_KT[pg]])
                    S.dma("gpsimd", lambda h: h.dma_start(out=vnew[:, :], in_=O["v_s"][4 * b:4 * b + 4, :]), reads=[b_vp], writes=[b_vnew])
                    def samp_A(hd, part):
                            par = hd % 2
                            Qb, b_Qb, scs, b_scs, Pbs, b_Pbs, st3, b_st3 = Qb2[par], b_Qb2[par], scs2[par], b_scs2[par], Pbs2[par], b_Pbs2[par], st32[par], b_st32[par]
                            if part == 1:
                                v_(lambda h: h.memset(Qb[:], 0.0), [], [b_Qb])
                                v_(lambda h: h.tensor_copy(Qb[0:64, 0:4], qTt[0:64, hd, 4 * b:4 * b + 4]), [b_qTt], [b_Qb])
                                v_(lambda h: h.tensor_copy(Qb[64:128, 4:8], qTt[64:128, hd, 4 * b:4 * b + 4]), [b_qTt], [b_Qb])
                                for cq in range(4):
                                    pp_, bpp = PS[(cq + 2 * par) % 4], bPS[(cq + 2 * par) % 4]
                                    t_(lambda h: h.matmul(pp_[0:8, :], Qb[:, :], KTall[:, 4 * cq:4 * cq + 4, hd, :], start=True, stop=True), [b_Qb] + b_KT[4 * cq:4 * cq + 4], [bpp])
                                    a_(lambda h: h.activation(scs[:, cq * 512:(cq + 1) * 512], pp_[0:8, :], AF.Copy, scale=0.125), [bpp], [b_scs])
                                t_(lambda h: h.matmul(PS[4][0:8, 0:80], Qb[:, :], kTt[:, hd, 0:80], start=True, stop=True), [b_Qb, b_kTt], [bPS[4]])
                                a_(lambda h: h.activation(scs[:, 2048:2052], PS[4][0:8, 4 * b:4 * b + 4], AF.Copy, scale=0.125), [bPS[4]], [b_scs])
                                v_(lambda h: h.tensor_tensor(scs[:, 1920:2052], scs[:, 1920:2052], Bs[:, hd, :], ALU.add), [b_scs, b_Bs], [b_scs])
                                v_(lambda h: h.tensor_reduce(st3[:, 0:1], scs[:, :], AX.X, ALU.max), [b_scs], [b_st3])
                                v_(lambda h: h.tensor_scalar(st3[:, 1:2], st3[:, 0:1], -1.0, None, ALU.mult), [b_st3], [b_st3])
                            if part == 2:
                                a_(lambda h: h.activation(Pbs[:, :], scs[:, :], AF.Exp, bias=st3[:, 1:2], scale=1.0, accum_out=st3[:, 2:3]), [b_scs, b_st3], [b_Pbs, b_st3])
                                v_(lambda h: h.reciprocal(st3[:, 3:4], st3[:, 2:3]), [b_st3], [b_st3])
                                v_(lambda h: h.tensor_scalar(Pbs[:, :], Pbs[:, :], st3[:, 3:4], None, ALU.mult), [b_Pbs, b_st3], [b_Pbs])
                    def samp_B(hd, part):
                            par = hd % 2
                            Pbs, b_Pbs, st3, b_st3, PTs, b_PTs, PTn, b_PTn = Pbs2[par], b_Pbs2[par], st32[par], b_st32[par], PTs2[par], b_PTs2[par], PTn2[par], b_PTn2[par]
                            c0 = 512 * par
                            if part == 1:
                                for pg in range(16):
                                    t_(lambda h: h.transpose(PSB[:, c0 + pg * 8:c0 + (pg + 1) * 8], Pbs[:, pg * 128:(pg + 1) * 128], ident_b[0:8, 0:8]), [b_Pbs, b_idb], [bPSBp[par], bPSB], inc=False)
                                t_(lambda h: h.transpose(PSB[0:4, c0 + 128:c0 + 136], Pbs[:, 2048:2052], ident_b[0:8, 0:8]), [b_Pbs, b_idb], [bPSBp[par], bPSB])
                                a_(lambda h: h.copy(PTs[:, :, :], PSB[:, c0:c0 + 128].rearrange("p (a b) -> p a b", a=16)), [bPSBp[par]], [b_PTs])
                                a_(lambda h: h.copy(PTn[:, :], PSB[0:4, c0 + 128:c0 + 136]), [bPSBp[par]], [b_PTn])
                            if part == 2:
                                po, bpo = PS[5], bPS[5]
                                for m in range(2):
                                    for pg in range(16):
                                        t_(lambda h: h.matmul(po[0:4, m * 128:(m + 1) * 128], PTs[:, pg, m * 4:(m + 1) * 4], Vpg(pg)[:, hd * 128:(hd + 1) * 128], start=(pg == 0), stop=False),
                                           [b_PTs, b_V[pg]], [bpo], inc=False)
                                    t_(lambda h: h.matmul(po[0:4, m * 128:(m + 1) * 128], PTn[:, m * 4:(m + 1) * 4], vnew[:, hd * 128:(hd + 1) * 128], start=False, stop=True),
                                       [b_PTn, b_vnew], [bpo], inc=(m == 1))
                                a_(lambda h: h.copy(o1s[:, :], po[0:4, 0:128]), [bpo], [b_o1s])
                                v_(lambda h: h.scalar_tensor_tensor(ods[:, :], po[0:4, 128:256], NLAM[0:4, :], o1s[:, :], ALU.mult, ALU.add), [bpo, b_small, b_o1s], [b_ods])
                                a_(lambda h: h.activation(junk[0:4, 0:128], ods[:, :], AF.Square, accum_out=st3[0:4, 5:6]), [b_ods], [bjunk, b_st3])
                                a_(lambda h: h.activation(st3[0:4, 6:7], st3[0:4, 5:6], AF.Ln, scale=1.0 / 128, bias=epsc[0:4, 0:1]), [b_st3, b_epsc], [b_st3])
                                a_(lambda h: h.activation(st3[0:4, 7:8], st3[0:4, 6:7], AF.Exp, scale=-0.5), [b_st3], [b_st3])
                                v_(lambda h: h.scalar_tensor_tensor(osbs[:, :], ods[:, :], st3[0:4, 7:8], swb[0:4, :], ALU.mult, ALU.mult), [b_ods, b_st3, b_swb], [b_osbs])
                                t_(lambda h: h.transpose(PSB[:, c0 + 256:c0 + 260], osbs[:, :], ident_b[0:4, 0:4]), [b_osbs, b_idb], [bPSBp[par], bPSB])
                                a_(lambda h: h.copy(oTs[:, hd, 4 * b:4 * b + 4], PSB[:, c0 + 256:c0 + 260]), [bPSBp[par]], [b_oTs])
                    samp_A(0, 1)
                    samp_A(0, 2)
                    for hd_ in range(8):
                        if hd_ + 1 < 8:
                            samp_A(hd_ + 1, 1)
                        samp_B(hd_, 1)
                        if hd_ + 1 < 8:
                            samp_A(hd_ + 1, 2)
                        samp_B(hd_, 2)
                for hd in range(8):
                    wo, b_wo = wload(I["at_w_out"][hd * 128:(hd + 1) * 128, :], 1, D)
                    for hf in range(2):
                        t_(lambda h: h.matmul(PS[2 + hf][0:64, :], oTs[:, hd, :], wo[:, 0, hf * 512:(hf + 1) * 512], start=(hd == 0), stop=(hd == 7)),
                           [b_oTs, b_wo], [bPS[2 + hf]], inc=(hd == 7))
                for hf in range(2):
                    xs_ = X[0:64, TAIL, hf * 512:(hf + 1) * 512]
                    v_(lambda h: h.tensor_tensor(xs_, xs_, PS[2 + hf][0:64, :], ALU.add), [bX[TAIL], bPS[2 + hf]], [bX[TAIL]])
                S.barrier()
            S.force_same = False

        def attn_phase():
            with ExitStack() as ph:
                load_wrow("mix1", 0)
                hTf = sb(ph, "hTf", [128, 8, NTOK], BF16); b_hTf = [Buf(f"ahTf{t}") for t in range(NT)]
                norm_all("mix1", hTf, b_hTf)
                if att_stop == 3:
                    S.barrier()
                    return
                qTt = sb(ph, "qTt", [128, 8, 80], BF16); b_qTt = Buf("qTt")
                kTt = sb(ph, "kTt", [128, 8, 80], BF16); b_kTt = Buf("kTt")
                small = sb(ph, "small", [128, 64], F32); b_small = Buf("small", True)
                swb = sb(ph, "swb", [128, 128], F32); b_swb = Buf("swb", True)
                ones_f = sb(ph, "ones_f", [128, 128], F32); b_ones = Buf("ones_f")
                ones_b = sb(ph, "ones_b", [128, 8], BF16); b_onesb = Buf("ones_b")
                v_(lambda h: h.memset(ones_f[:], 1.0), [], [b_ones])

                v_(lambda h: h.memset(ones_b[:], 1.0), [], [b_onesb])
                jflip = sb(ph, "jflipb", [128, 128], BF16); b_jflip = Buf("jflipb")
                su = ExitStack()
                lv = sb(su, "lv", [128, 4, 64], F32); b_lv = Buf("lv", True)
                for i, nm in enumerate(("at_lambda_q1", "at_lambda_k1", "at_lambda_q2", "at_lambda_k2")):
                    S.dma("sync", lambda h: h.dma_start(out=lv[:, i, :], in_=I[nm][0:1, :].partition_broadcast(128)), writes=[b_lv])
                v_(lambda h: h.tensor_tensor(lv[:, 0, :], lv[:, 0, :], lv[:, 1, :], ALU.mult), [b_lv], [b_lv])
                v_(lambda h: h.tensor_tensor(lv[:, 2, :], lv[:, 2, :], lv[:, 3, :], ALU.mult), [b_lv], [b_lv])
                v_(lambda h: h.tensor_reduce(small[:, 0:1], lv[:, 0, :], AX.X, ALU.add), [b_lv], [b_small])
                v_(lambda h: h.tensor_reduce(small[:, 1:2], lv[:, 2, :], AX.X, ALU.add), [b_lv], [b_small])
                a_(lambda h: h.activation(small[:, 2:4], small[:, 0:2], AF.Exp), [b_small], [b_small])
                v_(lambda h: h.tensor_tensor(small[:, 4:5], small[:, 3:4], small[:, 2:3], ALU.subtract), [b_small], [b_small])
                v_(lambda h: h.tensor_scalar(small[:, 5:6], small[:, 4:5], -LAMBDA_INIT, None, ALU.add), [b_small], [b_small])
                NLAM = small[:, 5:6]
                S.dma("sync", lambda h: h.dma_start(out=swb[:], in_=I["at_subln_w"][0:1, :].partition_broadcast(128)), writes=[b_swb])
                v_(lambda h: h.tensor_scalar(swb[:], swb[:], 1.0 - LAMBDA_INIT, None, ALU.mult), [b_swb], [b_swb])
                if att_stop == 4:
                    S.barrier()
                    return
                tabx = sb(su, "tabx", [64, 8], F32); b_tabx = Buf("tabx", True)
                tabh = sb(su, "tabh", [64, 2, 8], BF16); b_tabh = Buf("tabh", True)
                tabf = sb(su, "tabf", [64, 8], F32); b_tabf = Buf("tabf", True)
                oh = sb(su, "oh", [64, 384], F32); b_oh = Buf("oh", True)
                ohb = sb(su, "ohb", [64, 384], BF16); b_ohb = Buf("ohb", True)
                v_(lambda h: h.memset(tabx[:, :], NEGM), [], [b_tabx])
                S.dma("sync", lambda h: h.dma_start(out=tabx[0:32, :], in_=I["rel_bias_table"][:, :]), writes=[b_tabx])
                S.dma("sync", lambda h: h.dma_start(out=oh[0:33, :], in_=I["oh"][:, :]), writes=[b_oh])
                S.dma("gpsimd", lambda h: h.dma_start(out=jflip[:], in_=I["jflip"][:, :]), writes=[b_jflip])
                v_(lambda h: h.tensor_copy(ohb[0:33, :], oh[0:33, :]), [b_oh], [b_ohb])
                v_(lambda h: h.tensor_copy(tabh[:, 0, :], tabx[:, :]), [b_tabx], [b_tabh])
                v_(lambda h: h.tensor_copy(tabf[:, :], tabh[:, 0, :]), [b_tabh], [b_tabf])
                v_(lambda h: h.tensor_tensor(tabh[:, 1, :], tabx[:, :], tabf[:, :], ALU.subtract), [b_tabx, b_tabf], [b_tabh])
                wsb = sb(su, "wsb", [128, 3, 8], F32); b_wsb = Buf("wsb", True)
                t_(lambda h: h.matmul(PS[5][0:8, 0:384], tabh[0:33, 0, :], ohb[0:33, :], start=True, stop=False), [b_tabh, b_ohb], [bPS[5]], inc=False)
                t_(lambda h: h.matmul(PS[5][0:8, 0:384], tabh[0:33, 1, :], ohb[0:33, :], start=False, stop=True), [b_tabh, b_ohb], [bPS[5]])
                a_(lambda h: h.copy(junk[0:8, 0:384], PS[5][0:8, 0:384]), [bPS[5]], [bjunk])
                S.dma("sync", lambda h: h.dma_start(out=wscr[:, :], in_=junk[0:8, 0:384]), reads=[bjunk], writes=[b_wscr])
                for i3 in range(3):
                    t_(lambda h: h.matmul(PS[5][:, 400 + i3 * 8:408 + i3 * 8], ohb[0:33, i3 * 128:(i3 + 1) * 128], tabh[0:33, 0, :], start=True, stop=False),
                       [b_tabh, b_ohb], [bPS[5]], inc=False)
                    t_(lambda h: h.matmul(PS[5][:, 400 + i3 * 8:408 + i3 * 8], ohb[0:33, i3 * 128:(i3 + 1) * 128], tabh[0:33, 1, :], start=False, stop=True),
                       [b_tabh, b_ohb], [bPS[5]])
                a_(lambda h: h.copy(wsb[:, :, :], PS[5][:, 400:424].rearrange("p (a b) -> p a b", a=3)), [bPS[5]], [b_wsb])
                S.dma("sync", lambda h: h.dma_start(out=w2scr.rearrange("(a p) n -> p a n", p=128), in_=wsb[:, :, :]), reads=[b_wsb], writes=[b_w2scr])

                S.barrier()
                su.close()
                if att_stop == 1:
                    S.barrier()
                    return
                b_vp = Buf("dram_vp")
                with ExitStack() as kvp:
                    kvo = sb(kvp, "kvo", [128, 2, 512], F32); b_kvo = [Buf("kvo0"), Buf("kvo1")]
                    ctr = 0
                    for cbk in range(4):
                        wkk, bwkk = wload(I["at_w_qkv"][:, D + cbk * 512:D + (cbk + 1) * 512], 8, 512)
                        isv = cbk >= 2
                        ocs = slice((cbk % 2) * 512, (cbk % 2 + 1) * 512)
                        for t in range(NT):
                            r = rows(t)
                            j = ctr % 2
                            ctr += 1
                            pk, bpk = PS[2 + j], bPS[2 + j]
                            for k in range(8):
                                t_(lambda h: h.matmul(pk[:r, :], hTf[:, k, tcols(t)], wkk[:, k, :], start=(k == 0), stop=(k == 7)), [bwkk, b_hTf[t]], [bpk], inc=(k == 7))
                            a_(lambda h: h.copy(kvo[:r, j, :], pk[:r, :]), [bpk], [b_kvo[j]])
                            dp = O["v_p"] if isv else O["k_p"]
                            ds_ = O["v_s"] if isv else O["k_s"]
                            wr_ = [b_vp] if isv else [obuf]
                            if t < 16:
                                S.dma("sync", lambda h: h.dma_start(out=dp[16 + t * 128:16 + (t + 1) * 128, ocs], in_=kvo[:, j, :]), reads=[b_kvo[j]], writes=wr_, sembuf=b_kvo[j])
                            else:
                                S.dma("sync", lambda h: h.dma_start(out=ds_[:, ocs], in_=kvo[0:64, j, :]), reads=[b_kvo[j]], writes=wr_, sembuf=b_kvo[j])
                                S.dma("sync", lambda h: h.dma_start(out=dp[0:16, ocs], in_=kvo[64:80, j, :]), reads=[b_kvo[j]], writes=wr_, sembuf=b_kvo[j])
                    S.barrier()
                with ExitStack() as pp:
                    qTh = sb(pp, "qTh", [128, NTOK], BF16); b_qTh = Buf("qTh")
                    kTh = sb(pp, "kTh", [128, NTOK], BF16); b_kTh = Buf("kTh")
                    vh = sb(pp, "vh", [128, NT + 1, 128], BF16); b_vh = Buf("vh")
                    vhf = vh[:, :, :].rearrange("p a b -> p (a b)")
                    vmeta = sb(pp, "vmeta", [16, 256], BF16); b_vmeta = Buf("vmeta")
                    v_(lambda h: h.memset(vh[:], 0.0), [], [b_vh])
                    v_(lambda h: h.memset(vmeta[:], 0.0), [], [b_vmeta])
                    sc2 = [sb(pp, f"sc{m}", [128, 2064], F32) for m in range(2)]; b_sc2 = [Buf("sc0"), Buf("sc1")]
                    Pb2 = [sb(pp, f"Pb{m}", [128, 2064], BF16) for m in range(2)]; b_Pb2 = [Buf("Pb0"), Buf("Pb1")]
                    PT2 = [sb(pp, f"PT{m}", [128, 16, 128], BF16) for m in range(2)]; b_PT2 = [Buf("PT0"), Buf("PT1")]
                    PTm2 = [sb(pp, f"PTm{m}", [16, 128], BF16) for m in range(2)]; b_PTm2 = [Buf("PTm0", True), Buf("PTm1", True)]
                    st22 = [sb(pp, f"st2{m}", [128, 16], F32) for m in range(2)]; b_st22 = [Buf("st20", True), Buf("st21", True)]
                    sc, b_sc = sc2[1], b_sc2[1]
                    oTh = sb(pp, "oTh", [128, 2048], BF16); b_oTh = Buf("oTh")
                    Bh = sb(pp, "Bh", [128, 256], F32); b_Bh = Buf("Bh", True)
                    Bf = sb(pp, "Bf", [128, 272], F32); b_Bf = Buf("Bf", True)
                    Bfh = sb(pp, "Bfh", [128, 2, 272], BF16); b_Bfh = Buf("Bfh", True)
                    Bm0 = sb(pp, "Bm0", [128, 16], F32); b_Bm0 = Buf("Bm0", True)
                    o1 = sb(pp, "o1", [128, 128], F32); b_o1 = Buf("o1", True)
                    od = sb(pp, "od", [128, 128], F32); b_od = Buf("od", True)
                    osb = sb(pp, "osb", [128, 128], BF16); b_osb = Buf("osb", True)
                    st2 = sb(pp, "st2", [128, 16], F32); b_st2 = Buf("st2", True)
                    Wq = I["at_w_qkv"]
                    for hd in range(8):
                        wq, bwq = wload(Wq[:, hd * 128:(hd + 1) * 128], 8, 128)
                        wk, bwk = wload(Wq[:, D + hd * 128:D + (hd + 1) * 128], 8, 128)
                        S.dma("sync", lambda h: h.dma_start(out=Bf[:, 0:256], in_=dram_ap(wscr, hd * 384, [[1, 128], [1, 256]])), reads=[b_wscr], writes=[b_Bf])
                        S.dma("sync", lambda h: h.dma_start(out=Bf[:, 256:272], in_=dram_ap(wscr, hd * 384 + 112, [[1, 128], [1, 16]])), reads=[b_wscr], writes=[b_Bf])
                        v_(lambda h: h.tensor_copy(Bfh[:, 0, :], Bf[:, :]), [b_Bf], [b_Bfh])
                        v_(lambda h: h.tensor_tensor(Bfh[:, 1, :], Bf[:, :], Bfh[:, 0, :], ALU.subtract), [b_Bf, b_Bfh], [b_Bfh])
                        t_(lambda h: h.matmul(PS[5][:, 0:272], jflip[:, :], Bfh[:, 0, :], start=True, stop=False), [b_jflip, b_Bfh], [bPS[5]], inc=False)
                        t_(lambda h: h.matmul(PS[5][:, 0:272], jflip[:, :], Bfh[:, 1, :], start=False, stop=True), [b_jflip, b_Bfh], [bPS[5]])
                        a_(lambda h: h.copy(Bh[:, :], PS[5][:, 0:256]), [bPS[5]], [b_Bh])
                        a_(lambda h: h.copy(Bm0[:, :], PS[5][:, 256:272]), [bPS[5]], [b_Bm0])
                        if att_stop == 5:
                            break
                        TBA = [(0, 512), (512, 512), (1024, 512), (1536, 512), (1872, 256)]
                        for bi, (cb, n) in enumerate(TBA):
                            rd = b_hTf[cb // 128:(cb + n + 127) // 128]
                            for (wsrc, bws, pi) in ((wq, bwq, 0), (wk, bwk, 1)):
                                pp_, bpp = PS[pi], bPS[pi]
                                for k in range(8):
                                    t_(lambda h: h.matmul(pp_[:, :n], wsrc[:, k, :], hTf[:, k, cb:cb + n], start=(k == 0), stop=(k == 7)), [bws] + rd, [bpp], inc=(k == 7))
                                if pi == 0:
                                    if bi < 4:
                                        a_(lambda h: h.copy(qTh[:, cb:cb + n], pp_[:, :n]), [bpp], [b_qTh])
                                    else:
                                        v_(lambda h: h.tensor_copy(qTt[:, hd, :], pp_[:, 176:256]), [bpp], [b_qTt])
                                else:
                                    if bi < 4:
                                        a_(lambda h: h.copy(kTh[:, 16 + cb:16 + cb + n], pp_[:, :n]), [bpp], [b_kTh])
                                    else:
                                        a_(lambda h: h.copy(kTh[:, 0:16], pp_[:, 240:256]), [bpp], [b_kTh])
                                        v_(lambda h: h.tensor_copy(kTt[:, hd, :], pp_[:, 176:256]), [bpp], [b_kTt])
                        if att_stop == 6:
                            break
                        cs_ = slice(hd * 128, (hd + 1) * 128)
                        S.dma("gpsimd", lambda h: h.dma_start(out=vh[:, 0:16, :], in_=O["v_p"][16:16 + 2048, cs_].rearrange("(t p) n -> p t n", p=128)), reads=[b_vp], writes=[b_vh])
                        S.dma("gpsimd", lambda h: h.dma_start(out=vmeta[:, 0:128], in_=O["v_p"][0:16, cs_]), reads=[b_vp], writes=[b_vmeta])
                        if att_stop == 2:
                            break
                        MC = 2048 + 64
                        def unit_A(qt, m, part):
                                L0 = (qt + 1) * 128
                                L = 16 + L0
                                nb_ = (L + 511) // 512
                                bsz = (L + nb_ - 1) // nb_
                                blocks = [(c, min(bsz, L - c)) for c in range(0, L, bsz)]
                                ms = slice(m * 64, (m + 1) * 64)
                                scm, b_scm, Pbm, b_Pbm, stm, b_stm = sc2[m], b_sc2[m], Pb2[m], b_Pb2[m], st22[m], b_st22[m]
                                if part == 1:
                                    for bi, (kc, n) in enumerate(blocks):
                                        pp_, bpp = PS[(bi + 2 * m) % 4], bPS[(bi + 2 * m) % 4]
                                        t_(lambda h: h.matmul(pp_[:, :n], qTh[ms, qt * 128:(qt + 1) * 128], kTh[ms, kc:kc + n], start=True, stop=True), [b_qTh, b_kTh], [bpp])
                                        a_(lambda h: h.activation(scm[:, kc:kc + n], pp_[:, :n], AF.Copy, scale=0.125), [bpp], [b_scm])
                                    if qt == 0:
                                        v_(lambda h: h.tensor_tensor(scm[:, 0:16], scm[:, 0:16], Bm0[:, :], ALU.add), [b_scm, b_Bm0], [b_scm])
                                        v_(lambda h: h.tensor_tensor(scm[:, 16:144], scm[:, 16:144], Bh[:, 128:256], ALU.add), [b_scm, b_Bh], [b_scm])
                                    else:
                                        v_(lambda h: h.tensor_tensor(scm[:, L - 256:L], scm[:, L - 256:L], Bh[:, :], ALU.add), [b_scm, b_Bh], [b_scm])
                                    v_(lambda h: h.tensor_reduce(stm[:, 0:1], scm[:, :L], AX.X, ALU.max), [b_scm], [b_stm])
                                    v_(lambda h: h.tensor_scalar(stm[:, 1:2], stm[:, 0:1], -1.0, None, ALU.mult), [b_stm], [b_stm])
                                if part == 2:
                                    a_(lambda h: h.activation(Pbm[:, :L], scm[:, :L], AF.Exp, bias=stm[:, 1:2], scale=1.0, accum_out=stm[:, 2:3]), [b_scm, b_stm], [b_Pbm, b_stm])
                                    v_(lambda h: h.reciprocal(stm[:, 3:4], stm[:, 2:3]), [b_stm], [b_stm])

                        def unit_B(qt, m, part):
                                L0 = (qt + 1) * 128
                                L = 16 + L0
                                nb_ = (L + 511) // 512
                                bsz = (L + nb_ - 1) // nb_
                                blocks = [(c, min(bsz, L - c)) for c in range(0, L, bsz)]
                                Pbm, b_Pbm, stm, b_stm, PTq, b_PTq, PTmm, b_PTmm = Pb2[m], b_Pb2[m], st22[m], b_st22[m], PT2[m], b_PT2[m], PTm2[m], b_PTm2[m]
                                if part == 1:
                                    t_(lambda h: h.transpose(PSB[:16, 896:1024], Pbm[:, 0:16], ident_b[:, :]), [b_Pbm, b_idb], [bPSB])
                                    a_(lambda h: h.copy(PTmm[:, :], PSB[:16, 896:1024]), [bPSB], [b_PTmm])
                                    for j0 in range(0, qt + 1, 7):
                                        nj = min(7, qt + 1 - j0)
                                        for jj in range(nj):
                                            t_(lambda h: h.transpose(PSB[:, jj * 128:(jj + 1) * 128], Pbm[:, 16 + (j0 + jj) * 128:16 + (j0 + jj + 1) * 128], ident_b[:, :]),
                                               [b_Pbm, b_idb], [bPSB], inc=(jj == nj - 1))
                                        if (j0 // 7) % 2 == 0:
                                            a_(lambda h: h.copy(PTq[:, j0:j0 + nj, :], PSB[:, 0:nj * 128].rearrange("p (a b) -> p a b", a=nj)), [bPSB], [b_PTq])
                                        else:
                                            v_(lambda h: h.tensor_copy(PTq[:, j0:j0 + nj, :], PSB[:, 0:nj * 128].rearrange("p (a b) -> p a b", a=nj)), [bPSB], [b_PTq])
                                if part == 2:
                                    po, bpo = PS[4 + m], bPS[4 + m]
                                    t_(lambda h: h.matmul(po[:, 0:256], PTmm[:, :], vmeta[:, :], start=True, stop=False), [b_PTmm, b_vmeta], [bpo], inc=False)
                                    for j in range(qt + 1):
                                        t_(lambda h: h.matmul(po[:, 0:256], PTq[:, j, :], vhf[:, j * 128:j * 128 + 256], start=False, stop=(j == qt)), [b_PTq, b_vh], [bpo], inc=(j == qt))
                                    if m == 0:
                                        v_(lambda h: h.tensor_scalar(o1[:, :], po[:, 0:128], stm[:, 3:4], None, ALU.mult), [bpo, b_stm], [b_o1])
                                    else:
                                        v_(lambda h: h.tensor_tensor(stm[:, 4:5], stm[:, 3:4], NLAM, ALU.mult), [b_stm, b_small], [b_stm])
                                        v_(lambda h: h.scalar_tensor_tensor(od[:, :], po[:, 0:128], stm[:, 4:5], o1[:, :], ALU.mult, ALU.add), [bpo, b_stm, b_o1], [b_od])

                        def unit_C(qt):
                                a_(lambda h: h.activation(junk[:, 0:128], od[:, :], AF.Square, accum_out=st2[:, 5:6]), [b_od], [bjunk, b_st2])
                                a_(lambda h: h.activation(st2[:, 6:7], st2[:, 5:6], AF.Ln, scale=1.0 / 128, bias=epsc[:, 0:1]), [b_st2, b_epsc], [b_st2])
                                a_(lambda h: h.activation(st2[:, 7:8], st2[:, 6:7], AF.Exp, scale=-0.5), [b_st2], [b_st2])
                                v_(lambda h: h.scalar_tensor_tensor(osb[:, :], od[:, :], st2[:, 7:8], swb[:, :], ALU.mult, ALU.mult), [b_od, b_st2, b_swb], [b_osb])
                                t_(lambda h: h.transpose(PSB[:, 0:128], osb[:, :], ident_b[:, :]), [b_osb, b_idb], [bPSB])
                                a_(lambda h: h.copy(oTh[:, qt * 128:(qt + 1) * 128], PSB[:, 0:128]), [bPSB], [b_oTh])

                        units = [(qt, m) for qt in range(16) for m in range(2)]
                        unit_A(*units[0], 1)
                        unit_A(*units[0], 2)
                        for ui, (qt, m) in enumerate(units):
                            nxt = units[ui + 1] if ui + 1 < len(units) else None
                            if nxt:
                                unit_A(*nxt, 1)
                            unit_B(qt, m, 1)
                            if nxt:
                                unit_A(*nxt, 2)
                            unit_B(qt, m, 2)
                            if m == 1:
                                unit_C(qt)
                        if hd == 7:
                            dump("oTh7", oTh[:, :], [128, 2048], [b_oTh], BF16)
                            dump("sc7", sc[:, :], [128, 2064], [b_sc])
                            dump("Bh7", Bh[:, :], [128, 256], [b_Bh])
                            dump("Bm07", Bm0[:, :], [128, 16], [b_Bm0])
                            dump("st27", st2[:, :], [128, 16], [b_st2])
                            dump("small", small[:, :], [128, 64], [b_small])
                        wo, b_wo = wload(I["at_w_out"][hd * 128:(hd + 1) * 128, :], 1, D)
                        for t in range(16):
                            for hf in range(2):
                                pz, bpz = PS[5], bPS[5]
                                t_(lambda h: h.matmul(pz[:, :], oTh[:, t * 128:(t + 1) * 128], wo[:, 0, hf * 512:(hf + 1) * 512], start=True, stop=True), [b_oTh, b_wo], [bpz])
                                xs_ = X[:, t, hf * 512:(hf + 1) * 512]
                                v_(lambda h: h.tensor_tensor(xs_, xs_, pz[:, :], ALU.add), [bX[t], bpz], [bX[t]])
                    S.barrier()
                if do_sample:
                    sample_pass(hTf, qTt, b_qTt, kTt, b_kTt, b_small, NLAM, swb, b_swb, b_vp)
                S.barrier()

        if stage >= 2 and not skip0:
            ffn_phase(0)

        if stage >= 3:
            attn_phase()
        if stage >= 4:
            ffn_phase(1)
        if stage >= 5:
            load_wrow("fin", 0)
            with ExitStack() as ph:
                yo = sb(ph, "yo", [128, 2, D], F32); b_yo = [Buf("yo0"), Buf("yo1")]
                for t in range(NT):
                    r = 64 if t == TAIL else 128
                    j = t % 2
                    ss = stat[:r, 0:1]
                    a_(lambda h: h.activation(junk[:r, :], X[:r, t, :], AF.Square, accum_out=ss), [bX[t]], [bjunk, bstat])
                    a_(lambda h: h.activation(stat[:r, 2:3], ss, AF.Ln, scale=1.0 / D, bias=epsc[:r, 0:1]), [bstat, b_epsc], [bstat])
                    a_(lambda h: h.activation(stat[:r, 3:4], stat[:r, 2:3], AF.Exp, scale=-0.5), [bstat], [bstat])
                    v_(lambda h: h.scalar_tensor_tensor(yo[:r, j, :], X[:r, t, :], stat[:r, 3:4], wrow[:r, WR["fin"], :], ALU.mult, ALU.mult),
                       [bX[t], bstat, b_wrow[WR["fin"]]], [b_yo[j]])
                    dst = O["y_s"][:, :] if t == TAIL else O["y_p"][t * 128:(t + 1) * 128, :]
                    S.dma("sync", lambda h: h.dma_start(out=dst, in_=yo[:r, j, :]), reads=[b_yo[j]], writes=[obuf], sembuf=b_yo[j])
                S.barrier()

        if dbg:
            for t in range(NT):
                S.dma("sync", lambda h: h.dma_start(out=O["dbg_x"][t * 128:(t + 1) * 128, :], in_=X[:, t, :]), reads=[bX[t]], writes=[obuf], sembuf=bX[t])
        S.barrier()
        print("sched stats", {n: (e.n_inst, e.n_wait) for n, e in S.E.items()}, "nsem", S.nsem)
    return nc


def make_in_maps(inputs):
    consts = host_consts()
    f = lambda a: np.ascontiguousarray(a, dtype=np.float32)
    shared = {
        "cache_k": f(inputs["cache_k"]).reshape(N_PHYS * 128, D),
        "cache_v": f(inputs["cache_v"]).reshape(N_PHYS * 128, D),
        "meta_tokens": f(inputs["meta_tokens"]),
        "norm_mix_w": f(inputs["norm_mix_w"]), "norm_ffn_w": f(inputs["norm_ffn_w"]),
        "hg_w_in": f(inputs["hg_w_in"][0]), "hg_lower_bound": f(inputs["hg_lower_bound"]),
        "hg_norm_w": f(inputs["hg_norm_w"]), "hg_w_out": f(inputs["hg_w_out"][0]),
        "at_w_qkv": f(inputs["at_w_qkv"][0]),
        "at_lambda_q1": f(inputs["at_lambda_q1"]), "at_lambda_k1": f(inputs["at_lambda_k1"]),
        "at_lambda_q2": f(inputs["at_lambda_q2"]), "at_lambda_k2": f(inputs["at_lambda_k2"]),
        "at_subln_w": f(inputs["at_subln_w"]), "at_w_out": f(inputs["at_w_out"][0]),
        "rel_bias_table": f(inputs["rel_bias_table"]),
        "ff_w_up": f(inputs["ff_w_up"][0]), "ff_w_down": f(inputs["ff_w_down"][0]),
        "moe_w_router": f(inputs["moe_w_router"][0]), "moe_b_router": f(inputs["moe_b_router"]),
        "moe_w_up": f(inputs["moe_w_up"][0]).reshape(8 * D, 2 * DFFE),
        "moe_w_down": f(inputs["moe_w_down"][0]).reshape(8 * DFFE, D),
        "final_norm_w": f(inputs["final_norm_w"]).reshape(1, D),
    }
    shared.update(consts)
    maps = []
    for c in range(NCORES):
        m = dict(shared)
        m["xp"] = f(inputs["x_prompt"][c])
        m["xs"] = f(inputs["x_sample"][16 * c:16 * c + 16]).reshape(64, D)
        m["state"] = f(inputs["state_hgrn"][0, 16 * c:16 * c + 16])
        m["ptab"] = np.ascontiguousarray(inputs["page_table"][16 * c:16 * c + 16], dtype=np.int32)
        maps.append(m)
    return maps


def kernel(**inputs):
    nc = build()
    maps = make_in_maps(inputs)
    res = run_bass_kernel_spmd(nc, maps, core_ids=list(range(NCORES)))
    R = res.results
    y_p = np.stack([R[c]["y_p"] for c in range(NCORES)])
    y_s = np.concatenate([R[c]["y_s"].reshape(16, 4, D) for c in range(NCORES)])
    st_p = np.stack([R[c]["st_p"] for c in range(NCORES)])[None]
    st_s = np.concatenate([R[c]["st_s"] for c in range(NCORES)])[None]
    k_p = np.stack([R[c]["k_p"].reshape(2064, 8, 2, 64) for c in range(NCORES)])[None]
    v_p = np.stack([R[c]["v_p"].reshape(2064, 8, 128) for c in range(NCORES)])[None]
    k_s = np.concatenate([R[c]["k_s"].reshape(16, 4, 8, 2, 64) for c in range(NCORES)])[None]
    v_s = np.concatenate([R[c]["v_s"].reshape(16, 4, 8, 128) for c in range(NCORES)])[None]
    return (y_p, y_s, st_p, st_s, k_p, v_p, k_s, v_s)
```
